# Optimizing a Trainium2 kernel written in Bass

```python
import math
import jax, jax.numpy as jnp
from jax import lax
import numpy as np

D_MODEL = 2048
BATCH = 2
SEQ = 4096
DEPTH = 1

CHUNK = 64
GDN_QK_HEADS = 16
GDN_V_HEADS = 32
GDN_HEAD_DIM = 128
GDN_CONV = 4
GDN_QK_DIM = GDN_QK_HEADS * GDN_HEAD_DIM
GDN_V_DIM = GDN_V_HEADS * GDN_HEAD_DIM
GDN_QKV_DIM = 2 * GDN_QK_DIM + GDN_V_DIM
V_PER_QK = GDN_V_HEADS // GDN_QK_HEADS
SC_WIDTH = D_MODEL
SC_CONV = 3
N_EXPERTS = 32
TOP_K = 4
D_FF = D_MODEL
SWIGLU_LIMIT = 7.0
SWIGLU_ALPHA = 1.702
EXPERT_BLOCK = 128
LN_EPS = 1e-5
RMS_EPS = 1e-6
DN_ALPHA = (2 * DEPTH) ** 0.25
DN_BETA = (8 * DEPTH) ** -0.25
SPLITS = (GDN_QKV_DIM, GDN_V_DIM, GDN_V_HEADS, GDN_V_HEADS, SC_WIDTH, SC_WIDTH, SC_WIDTH, D_MODEL, D_MODEL)
IN_PROJ_DIM = sum(SPLITS)

kernel_name = "hybrid_gdn_shortconv_moe_deepnorm"


def layer_norm(x, g, b):
    xf = x.astype(jnp.float32)
    mu = jnp.mean(xf, axis=-1, keepdims=True)
    var = jnp.mean(jnp.square(xf - mu), axis=-1, keepdims=True)
    return ((xf - mu) * lax.rsqrt(var + LN_EPS) * g + b).astype(x.dtype)


def l2_normalize(x):
    xf = x.astype(jnp.float32)
    return xf * lax.rsqrt(jnp.sum(xf * xf, axis=-1, keepdims=True) + RMS_EPS)


def causal_depthwise_conv(x, w):
    k_width, channels = w.shape
    xp = jnp.pad(x, ((0, 0), (k_width - 1, 0), (0, 0)))
    return lax.conv_general_dilated(xp, w[:, None, :].astype(x.dtype), window_strides=(1,), padding='VALID',
                                    dimension_numbers=('NWC', 'WIO', 'NWC'), feature_group_count=channels)


def gated_delta_rule(q, k, v, beta, g):
    f32 = jnp.float32
    bsz, seq, heads, dk = q.shape
    dv = v.shape[-1]
    n_chunks = seq // CHUNK

    def to_chunks(t):
        return t.astype(f32).reshape(bsz, n_chunks, CHUNK, heads, -1).transpose(0, 3, 1, 2, 4)

    q = to_chunks(q) * (dk ** -0.5)
    k = to_chunks(k)
    v = to_chunks(v)
    beta = to_chunks(beta[..., None])
    g = jnp.cumsum(to_chunks(g[..., None])[..., 0], axis=-1)

    incl = jnp.tril(jnp.ones((CHUNK, CHUNK), dtype=bool))
    strict = jnp.tril(jnp.ones((CHUNK, CHUNK), dtype=bool), -1)
    decay = jnp.exp(jnp.where(incl, g[..., :, None] - g[..., None, :], -jnp.inf))

    k_beta = k * beta
    lower = jnp.where(strict, jnp.einsum('bhnid,bhnjd->bhnij', k_beta, k) * decay, 0.0)
    eye = jnp.eye(CHUNK, dtype=f32)
    rhs = jnp.concatenate([v * beta, k_beta * jnp.exp(g)[..., None]], axis=-1)
    sol = lax.linalg.triangular_solve(eye + lower, rhs, left_side=True, lower=True, unit_diagonal=True)
    u, w = sol[..., :dv], sol[..., dv:]

    attn = jnp.where(incl, jnp.einsum('bhnid,bhnjd->bhnij', q, k) * decay, 0.0)
    q_dec = q * jnp.exp(g)[..., None]
    k_dec = k * jnp.exp(g[..., -1:] - g)[..., None]
    g_last = jnp.exp(g[..., -1])

    def step(state, xs):
        q_c, k_c, u_c, w_c, a_c, gl_c = xs
        v_new = u_c - jnp.einsum('bhck,bhkv->bhcv', w_c, state)
        o = jnp.einsum('bhck,bhkv->bhcv', q_c, state) + jnp.einsum('bhij,bhjv->bhiv', a_c, v_new)
        state = state * gl_c[..., None, None] + jnp.einsum('bhck,bhcv->bhkv', k_c, v_new)
        return state, o

    lead = lambda t: jnp.moveaxis(t, 2, 0)
    state0 = jnp.zeros((bsz, heads, dk, dv), f32)
    _, o = lax.scan(step, state0, (lead(q_dec), lead(k_dec), lead(u), lead(w), lead(attn), lead(g_last)))
    return o.transpose(1, 0, 3, 2, 4).reshape(bsz, seq, heads, dv)


def gated_rms_norm(o, z, w):
    zf = z.astype(jnp.float32)
    var = jnp.mean(o * o, axis=-1, keepdims=True)
    return (o * lax.rsqrt(var + RMS_EPS) * w * jax.nn.silu(zf)).astype(z.dtype)


def hybrid_mixer(x, w_in, gdn_conv_w, gdn_a_log, gdn_dt_bias, gdn_norm_w, w_out_gdn, sc_conv_w, w_out_sc, w_out):
    bsz, seq, _ = x.shape
    proj = x @ w_in
    offs = np.cumsum(SPLITS)[:-1].tolist()
    qkv, z, b_raw, a_raw, sc_b, sc_c, sc_x, gate_a, gate_b = jnp.split(proj, offs, axis=-1)

    qkv = jax.nn.silu(causal_depthwise_conv(qkv, gdn_conv_w))
    q, k, v = jnp.split(qkv, [GDN_QK_DIM, 2 * GDN_QK_DIM], axis=-1)
    q = jnp.repeat(l2_normalize(q.reshape(bsz, seq, GDN_QK_HEADS, GDN_HEAD_DIM)), V_PER_QK, axis=2)
    k = jnp.repeat(l2_normalize(k.reshape(bsz, seq, GDN_QK_HEADS, GDN_HEAD_DIM)), V_PER_QK, axis=2)
    v = v.reshape(bsz, seq, GDN_V_HEADS, GDN_HEAD_DIM)
    beta = jax.nn.sigmoid(b_raw.astype(jnp.float32))
    g = -jnp.exp(gdn_a_log.astype(jnp.float32)) * jax.nn.softplus(a_raw.astype(jnp.float32) + gdn_dt_bias.astype(jnp.float32))
    o = gated_delta_rule(q, k, v, beta, g)
    o = gated_rms_norm(o, z.reshape(bsz, seq, GDN_V_HEADS, GDN_HEAD_DIM), gdn_norm_w)
    y_a = o.reshape(bsz, seq, GDN_V_DIM) @ w_out_gdn

    y_b = (sc_b * causal_depthwise_conv(sc_c * sc_x, sc_conv_w)) @ w_out_sc

    merged = jax.nn.sigmoid(gate_a) * y_a + jax.nn.sigmoid(gate_b) * y_b
    return merged @ w_out


def moe_ffn(h, w_router, b_router, w_gate_up, b_gate_up, w_down, b_down):
    n_tok = h.shape[0]
    n_assign = n_tok * TOP_K
    logits = (h @ w_router + b_router).astype(jnp.float32)
    top_val, top_idx = lax.top_k(logits, TOP_K)
    gate = jax.nn.softmax(top_val, axis=-1)

    flat_e = top_idx.reshape(-1)
    flat_w = gate.reshape(-1)
    order = jnp.argsort(flat_e)
    sorted_e = flat_e[order]
    token_of = order // TOP_K
    counts = jax.ops.segment_sum(jnp.ones_like(flat_e), flat_e, num_segments=N_EXPERTS)
    padded = (counts + EXPERT_BLOCK - 1) // EXPERT_BLOCK * EXPERT_BLOCK
    start = jnp.cumsum(counts) - counts
    pend = jnp.cumsum(padded)
    pstart = pend - padded
    dest = pstart[sorted_e] + (jnp.arange(n_assign) - start[sorted_e])

    n_blocks = -(-n_assign // EXPERT_BLOCK) + N_EXPERTS
    rows = jnp.zeros((n_blocks * EXPERT_BLOCK, h.shape[1]), h.dtype).at[dest].set(h[token_of])
    block_e = jnp.minimum(jnp.searchsorted(pend, jnp.arange(n_blocks) * EXPERT_BLOCK, side='right'), N_EXPERTS - 1)

    def expert_block(args):
        xb, e = args
        gu = xb @ w_gate_up[e] + b_gate_up[e]
        gt = jnp.minimum(gu[:, 0::2], SWIGLU_LIMIT)
        up = jnp.clip(gu[:, 1::2], -SWIGLU_LIMIT, SWIGLU_LIMIT)
        act = (up + 1.0) * (gt * jax.nn.sigmoid(SWIGLU_ALPHA * gt))
        return act @ w_down[e] + b_down[e]

    y = lax.map(expert_block, (rows.reshape(n_blocks, EXPERT_BLOCK, -1), block_e))
    y = y.reshape(n_blocks * EXPERT_BLOCK, -1)[dest]
    weighted = y * flat_w[order][:, None].astype(y.dtype)
    return jax.ops.segment_sum(weighted, token_of, num_segments=n_tok)


def setup_inputs(seed: int = 0) -> dict:
    key = jax.random.key(seed)
    ks = jax.random.split(key, 24)
    f32 = jnp.float32
    nrm = lambda k, shape, scale: jax.random.normal(k, shape, f32) * scale
    L = DEPTH
    dt = jnp.exp(jax.random.uniform(ks[6], (L, GDN_V_HEADS), f32, math.log(1e-3), math.log(1e-1)))
    return {
        "x": nrm(ks[0], (BATCH, SEQ, D_MODEL), 1.0),
        "ln_in_g": 1.0 + nrm(ks[1], (D_MODEL,), 0.02),
        "ln_in_b": nrm(ks[2], (D_MODEL,), 0.02),
        "w_in": nrm(ks[3], (L, D_MODEL, IN_PROJ_DIM), D_MODEL ** -0.5),
        "gdn_conv_w": nrm(ks[4], (L, GDN_CONV, GDN_QKV_DIM), GDN_CONV ** -0.5),
        "gdn_a_log": jnp.log(jax.random.uniform(ks[5], (L, GDN_V_HEADS), f32, 1.0, 16.0)),
        "gdn_dt_bias": dt + jnp.log(-jnp.expm1(-dt)),
        "gdn_norm_w": 1.0 + nrm(ks[7], (L, GDN_HEAD_DIM), 0.02),
        "w_out_gdn": nrm(ks[8], (L, GDN_V_DIM, D_MODEL), GDN_V_DIM ** -0.5 * DN_BETA),
        "sc_conv_w": nrm(ks[9], (L, SC_CONV, SC_WIDTH), SC_CONV ** -0.5),
        "w_out_sc": nrm(ks[10], (L, SC_WIDTH, D_MODEL), SC_WIDTH ** -0.5 * DN_BETA),
        "w_out": nrm(ks[11], (L, D_MODEL, D_MODEL), D_MODEL ** -0.5 * DN_BETA),
        "ln_mix_g": 1.0 + nrm(ks[12], (L, D_MODEL), 0.02),
        "ln_mix_b": nrm(ks[13], (L, D_MODEL), 0.02),
        "w_router": nrm(ks[14], (L, D_MODEL, N_EXPERTS), D_MODEL ** -0.5),
        "b_router": nrm(ks[15], (L, N_EXPERTS), 0.01),
        "w_gate_up": nrm(ks[16], (L, N_EXPERTS, D_MODEL, 2 * D_FF), D_MODEL ** -0.5),
        "b_gate_up": nrm(ks[17], (L, N_EXPERTS, 2 * D_FF), 0.01),
        "w_down": nrm(ks[18], (L, N_EXPERTS, D_FF, D_MODEL), D_FF ** -0.5 * DN_BETA),
        "b_down": nrm(ks[19], (L, N_EXPERTS, D_MODEL), 0.01),
        "ln_ffn_g": 1.0 + nrm(ks[20], (L, D_MODEL), 0.02),
        "ln_ffn_b": nrm(ks[21], (L, D_MODEL), 0.02),
    }


def reference(x, ln_in_g, ln_in_b, w_in, gdn_conv_w, gdn_a_log, gdn_dt_bias, gdn_norm_w, w_out_gdn,
              sc_conv_w, w_out_sc, w_out, ln_mix_g, ln_mix_b, w_router, b_router, w_gate_up, b_gate_up,
              w_down, b_down, ln_ffn_g, ln_ffn_b):
    bsz, seq, _ = x.shape
    h = layer_norm(x, ln_in_g, ln_in_b)
    for l in range(DEPTH):
        mix = hybrid_mixer(h, w_in[l], gdn_conv_w[l], gdn_a_log[l], gdn_dt_bias[l], gdn_norm_w[l],
                           w_out_gdn[l], sc_conv_w[l], w_out_sc[l], w_out[l])
        h = layer_norm(DN_ALPHA * h + mix, ln_mix_g[l], ln_mix_b[l])
        ffn = moe_ffn(h.reshape(bsz * seq, D_MODEL), w_router[l], b_router[l], w_gate_up[l], b_gate_up[l],
                      w_down[l], b_down[l]).reshape(bsz, seq, D_MODEL)
        h = layer_norm(DN_ALPHA * h + ffn, ln_ffn_g[l], ln_ffn_b[l])
    return h
```

```python
import numpy as np
import concourse.bass as bass
import concourse.mybir as mybir
from concourse.bass_utils import run_bass_kernel_spmd

F32 = mybir.dt.float32
BF16 = mybir.dt.bfloat16
AF = mybir.ActivationFunctionType
ALU = mybir.AluOpType
AX = mybir.AxisListType

SEM_LIMIT = 8000
_ESZ = {F32: 4, BF16: 2, mybir.dt.int32: 4, mybir.dt.uint32: 4}


class _Op:
    __slots__ = ("eng", "fn", "deps", "idx", "sig", "semval", "dsem", "dval", "ring_wait")

    def __init__(self, eng, fn):
        self.eng = eng
        self.fn = fn
        self.deps = set()
        self.sig = False
        self.semval = 0
        self.dsem = None
        self.dval = 0
        self.ring_wait = None


def _region(ap):
    t = ap.tensor
    space = str(ap.space) if hasattr(ap, "space") else ""
    esz = _ESZ.get(ap.dtype, 4)
    dims = ap.ap
    if "DRAM" in space.upper() or "HBM" in space.upper() or type(t).__name__.startswith("DRam"):
        lo = ap.offset
        hi = lo + sum((c - 1) * s for s, c in dims)
        return (t.name, 0, 1, lo * esz, (hi + 1) * esz)
    if type(t).__name__.startswith("PSum"):
        return (t.name, 0, 128, 0, 1 << 30)
    pstep = dims[0][0]
    pcount = dims[0][1]
    rowel = 1
    for s in t.shape[1:]:
        rowel *= s
    tesz = _ESZ.get(t.dtype, 4)
    rowel = rowel * tesz // esz
    if pcount == 1 and pstep != rowel:
        pstep = rowel
    p0 = ap.offset // rowel
    f0 = ap.offset % rowel
    span = sum((c - 1) * s for s, c in dims[1:])
    return (t.name, p0, p0 + pcount, f0 * esz, (f0 + span + 1) * esz)


class Sched:
    COMPUTE = ("pe", "act", "dve", "pool")

    def __init__(self, nc, dma_ring=16):
        self.nc = nc
        self.ops = []
        self.by_eng = {}
        self.track = {}
        self.dma_ring = dma_ring
        self.dma_count = {}
        self.untracked = set()
        self.barrier_deps = set()
        self.barrier_id = 0
        self.eng_barrier = {}

    def add(self, eng, fn, reads=(), writes=()):
        op = _Op(eng, fn)
        op.idx = len(self.ops)
        self.ops.append(op)
        self.by_eng.setdefault(eng, []).append(op)
        for ap in reads:
            self._access(op, ap, False)
        for ap in writes:
            self._access(op, ap, True)
        hwk = {"qact": "act", "qpool": "pool"}.get(eng, eng)
        if self.eng_barrier.get(hwk, 0) < self.barrier_id:
            self.eng_barrier[hwk] = self.barrier_id
            op.deps |= {d for d in self.barrier_deps if d != op.idx}
        if eng.startswith("q"):
            n = self.dma_count.get(eng, 0)
            self.dma_count[eng] = n + 1
            op.dsem = n % self.dma_ring
            op.dval = 16 * (n // self.dma_ring + 1)
            if n >= self.dma_ring:
                op.ring_wait = (op.dsem, op.dval - 16)
        return op

    def barrier(self):
        deps = set()
        for e, lst in self.by_eng.items():
            if not lst:
                continue
            if e.startswith("q"):
                deps |= {o.idx for o in lst[-self.dma_ring:]}
            else:
                deps.add(lst[-1].idx)
        self.barrier_deps = deps
        self.barrier_id += 1
        self.track = {}

    def _access(self, op, ap, is_write):
        name, p0, p1, f0, f1 = _region(ap)
        if name in self.untracked:
            return
        if f1 == (1 << 30):
            is_write = True
        recs = self.track.setdefault(name, [])
        keep = []
        for r in recs:
            ov = not (r[1] <= p0 or p1 <= r[0] or r[3] <= f0 or f1 <= r[2])
            if not ov:
                keep.append(r)
                continue
            if r[4] != op.idx and (is_write or r[5]):
                op.deps.add(r[4])
            covered = p0 <= r[0] and r[1] <= p1 and f0 <= r[2] and r[3] <= f1
            if is_write and covered:
                continue
            if (not is_write) and (not r[5]) and covered and r[6] == op.eng and not op.eng.startswith("q"):
                continue
            keep.append(r)
        keep.append([p0, p1, f0, f1, op.idx, is_write, op.eng])
        self.track[name] = keep

    def emit(self):
        nc = self.nc
        ops = self.ops
        for op in ops:
            nd = set()
            for d in op.deps:
                dop = ops[d]
                if dop.eng == op.eng and op.eng == "pe":
                    continue
                nd.add(d)
                if not dop.eng.startswith("q"):
                    dop.sig = True
            op.deps = nd
        for e, lst in self.by_eng.items():
            if e.startswith("q"):
                continue
            c = 0
            for op in lst:
                if op.sig:
                    op.semval = (c // SEM_LIMIT, c % SEM_LIMIT + 1)
                    c += 1
            self.nsig = getattr(self, "nsig", {})
            self.nsig[e] = c
        from contextlib import ExitStack

        with ExitStack() as st:
            st.enter_context(nc.cleanup_on_exit())
            csem = {
                e: [nc.alloc_semaphore("s_%s_%d" % (e, i)) for i in range(self.nsig[e] // SEM_LIMIT + 1)]
                for e in self.by_eng
                if not e.startswith("q")
            }
            dsem = {
                e: [nc.alloc_semaphore("d_%s_%d" % (e, i)) for i in range(min(self.dma_ring, self.dma_count[e]))]
                for e in self.by_eng
                if e.startswith("q")
            }
            for lst_ in list(csem.values()) + list(dsem.values()):
                for s_ in lst_:
                    nc.gpsimd.sem_clear(s_)
            nc.all_engine_barrier()
            st.callback(nc.all_engine_barrier)
            block = st.enter_context(nc.Block())
            engmap = {
                "pe": "tensor",
                "act": "scalar",
                "dve": "vector",
                "pool": "gpsimd",
                "qsp": "sync",
                "qact": "scalar",
                "qpool": "gpsimd",
            }
            hw = {}
            for op in ops:
                hw.setdefault(engmap[op.eng], []).append(op)

            def make(hwname, lst):
                def body(eng):
                    waited = {}

                    def w(key, sem, val):
                        if waited.get(key, 0) >= val:
                            return
                        waited[key] = val
                        eng.wait_ge(sem, val)

                    for op in lst:
                        if op.ring_wait is not None:
                            w((op.eng, op.ring_wait[0]), dsem[op.eng][op.ring_wait[0]], op.ring_wait[1])
                        for d in sorted(op.deps):
                            dop = ops[d]
                            if dop.eng.startswith("q"):
                                w((dop.eng, dop.dsem), dsem[dop.eng][dop.dsem], dop.dval)
                            else:
                                w((dop.eng, dop.semval[0]), csem[dop.eng][dop.semval[0]], dop.semval[1])
                        ins = op.fn(eng)
                        if op.eng.startswith("q"):
                            ins.then_inc(dsem[op.eng][op.dsem], 16)
                        elif op.sig:
                            ins.then_inc(csem[op.eng][op.semval[0]], 1)
                    for qe in dsem:
                        if engmap[qe] != hwname:
                            continue
                        n = self.dma_count[qe]
                        for i, s in enumerate(dsem[qe]):
                            uses = (n - i + self.dma_ring - 1) // self.dma_ring
                            if uses > 0:
                                w((qe, i), s, 16 * uses)

                return body

            for hwname, lst in hw.items():
                getattr(block, hwname)(make(hwname, lst))

    def dma(self, q, out, in_, **kw):
        return self.add(q, lambda e: e.dma_start(out=out, in_=in_, **kw), reads=[in_], writes=[out])

    def mm(self, out, lhsT, rhs, start=True, stop=True):
        rd = [lhsT, rhs] + ([] if start else [out])
        return self.add("pe", lambda e: e.matmul(out, lhsT, rhs, start=start, stop=stop), reads=rd, writes=[out])

    def tr(self, out, in_, ident):
        return self.add("pe", lambda e: e.transpose(out, in_, ident), reads=[in_, ident], writes=[out])

    def act(self, out, in_, func, bias=None, scale=1.0, accum_out=None, eng="act"):
        rd = [in_]
        kw = {}
        if bias is not None:
            kw["bias"] = bias
            if not isinstance(bias, (int, float)):
                rd.append(bias)
        if not isinstance(scale, (int, float)):
            rd.append(scale)
        wr = [out]
        if accum_out is not None:
            kw["accum_out"] = accum_out
            wr.append(accum_out)
        return self.add(eng, lambda e: e.activation(out, in_, func, scale=scale, **kw), reads=rd, writes=wr)

    def tt(self, eng, out, in0, in1, op):
        return self.add(eng, lambda e: e.tensor_tensor(out, in0, in1, op), reads=[in0, in1], writes=[out])

    def ts(self, eng, out, in0, s1, s2, op0, op1=None, accum_out=None):
        rd = [in0] + [s for s in (s1, s2) if s is not None and not isinstance(s, (int, float))]
        wr = [out] + ([accum_out] if accum_out is not None else [])
        kw = {}
        if accum_out is not None:
            kw["accum_out"] = accum_out
        if op1 is None:
            return self.add(eng, lambda e: e.tensor_scalar(out, in0, s1, None, op0, **kw), reads=rd, writes=wr)
        return self.add(eng, lambda e: e.tensor_scalar(out, in0, s1, s2, op0, op1, **kw), reads=rd, writes=wr)

    def stt(self, eng, out, in0, scalar, in1, op0, op1):
        rd = [in0, in1] + ([] if isinstance(scalar, (int, float)) else [scalar])
        return self.add(eng, lambda e: e.scalar_tensor_tensor(out, in0, scalar, in1, op0, op1), reads=rd, writes=[out])

    def copy(self, eng, out, in_):
        if eng == "act":
            return self.add(eng, lambda e: e.copy(out, in_), reads=[in_], writes=[out])
        return self.add(eng, lambda e: e.tensor_copy(out, in_), reads=[in_], writes=[out])

    def memset(self, eng, ap, val):
        return self.add(eng, lambda e: e.memset(ap, val), reads=[], writes=[ap])


class Pool:
    def __init__(self, st, nc, name, shape, dtype, n, psum=False):
        mk = nc.psum_tensor if psum else nc.sbuf_tensor
        self.t = [st.enter_context(mk("%s%d" % (name, i), shape, dtype)) for i in range(n)]
        self.i = 0

    def get(self):
        t = self.t[self.i % len(self.t)]
        self.i += 1
        return t


D = 2048
KC = 16
SEQ = 4096
NCHK = 32
NH = 32
OWN = 1024
EXT = 1152
NE = 32
ALPHA = 2.0 ** 0.25
LN_EPS = 1e-5
RMS_EPS = 1e-6
C_Q, C_K, C_V, C_Z, C_B, C_A, C_SB, C_SC, C_SX, C_GA, C_GB = 0, 2048, 4096, 8192, 12288, 12320, 12352, 14400, 16448, 18496, 20544

DEBUG = False
PHASES = (0, 1, 2, 3)
GDN_GROUPS = 16


def ln_stats(S, xt, n, st_pool, sq_pool):
    stt = st_pool.get()
    sq = sq_pool.get()
    S.add("dve", lambda e: e.reduce_sum(stt[:, 0:1], xt, AX.X), reads=[xt], writes=[stt[:, 0:1]])
    sqv = sq[:, 0:n]
    S.act(sqv, xt, AF.Square)
    S.add("dve", lambda e: e.reduce_sum(stt[:, 1:2], sqv, AX.X), reads=[sqv], writes=[stt[:, 1:2]])
    S.ts("dve", stt[:, 2:3], stt[:, 0:1], 1.0 / n, None, ALU.mult)
    S.tt("dve", stt[:, 3:4], stt[:, 2:3], stt[:, 2:3], ALU.mult)
    S.stt("dve", stt[:, 4:5], stt[:, 1:2], 1.0 / n, stt[:, 3:4], ALU.mult, ALU.subtract)
    S.ts("dve", stt[:, 4:5], stt[:, 4:5], LN_EPS, None, ALU.add)
    S.act(stt[:, 5:6], stt[:, 4:5], AF.Sqrt)
    S.add("dve", lambda e: e.reciprocal(stt[:, 6:7], stt[:, 5:6]), reads=[stt[:, 5:6]], writes=[stt[:, 6:7]])
    S.stt("dve", stt[:, 7:8], stt[:, 2:3], -1.0, stt[:, 6:7], ALU.mult, ALU.mult)
    return stt[:, 6:7], stt[:, 7:8]


class Slots:
    def __init__(self, st, nc, name, dtype, nbanks, per_bank, psum=True):
        mk = nc.psum_tensor if psum else nc.sbuf_tensor
        self.s = []
        ts = [st.enter_context(mk("%s%d" % (name, b), [128, per_bank, 128], dtype)) for b in range(nbanks)]
        for k in range(per_bank):
            for t in ts:
                self.s.append(t[:, k, :])
        self.i = 0

    def get(self):
        s = self.s[self.i % len(self.s)]
        self.i += 1
        return s


def phase0(S, nc, A, P):
    from contextlib import ExitStack
    import os

    with ExitStack() as st:
        xt_pool = Pool(st, nc, "xt", [128, D], F32, 2)
        sq_pool = Pool(st, nc, "sq", [128, D], F32, 1)
        st_pool = Pool(st, nc, "lst", [128, 8], F32, 3)
        xn_pool = Pool(st, nc, "xn", [128, D], BF16, 2)
        hT_pool = Pool(st, nc, "hT", [128, KC, 128], BF16, 2)
        h0_pool = Pool(st, nc, "h0t", [128, D], F32, 2)
        ptr = Pool(st, nc, "ptr", [128, 8, 128], BF16, 2, psum=True)
        pba = Pool(st, nc, "pba", [128, 512], F32, 2, psum=True)
        wba = st.enter_context(nc.sbuf_tensor("wba", [128, KC, 64], BF16))
        if os.environ.get("KNOW", "0") != "1":
            S.dma("qpool", wba[:], A["w_in_v"][:, :, C_B : C_B + 64])
        gbi = st.enter_context(nc.sbuf_tensor("gbi", [128, 2, D], F32))
        S.dma("qsp", gbi[:], A["gb_in"])
        STOP = int(os.environ.get("KSTOP", "99"))
        for src, ntiles, dstT, ext in ((A["xb"], NCHK, A["h0T_d"], False), (A["xe"], EXT // 128, A["h0Te_d"], True)):
            if STOP < 2:
                break
            for t in range(ntiles if STOP > 10 else 1):
                xt = xt_pool.get()
                S.dma("qsp", xt[:], src[t * 128 : (t + 1) * 128, :])
                rstd, nmr = ln_stats(S, xt[:], D, st_pool, sq_pool)
                xn = xn_pool.get()
                S.act(xn[:], xt[:], AF.Identity, bias=nmr, scale=rstd)
                if STOP < 3:
                    continue
                hT = hT_pool.get()
                for half in range(2):
                    pt = ptr.get()
                    for c in range(8):
                        cc = half * 8 + c
                        S.tr(pt[:, c, :], xn[:, cc * 128 : (cc + 1) * 128], P["identb"][:])
                    for c in range(8):
                        cc = half * 8 + c
                        S.act(hT[:, cc, :], pt[:, c, :], AF.Identity, bias=P["lnin"][:, 1, cc : cc + 1], scale=P["lnin"][:, 0, cc : cc + 1])
                if STOP < 4:
                    continue
                S.dma("qsp", dstT.rearrange("c p t -> p c t")[:, :, t * 128 : (t + 1) * 128], hT[:])
                if STOP < 5:
                    continue
                if not ext:
                    pb = pba.get()
                    for c in range(KC):
                        S.mm(pb[:, 0:64], hT[:, c, :], wba[:, c, :], start=(c == 0), stop=(c == KC - 1))
                    S.copy("act", P["ba_tok"][:, t, :], pb[:, 0:64])
                else:
                    h0t = h0_pool.get()
                    S.act(h0t[:], xt[:], AF.Identity, bias=nmr, scale=rstd)
                    S.tt("dve", h0t[:], h0t[:], gbi[:, 0, :], ALU.mult)
                    S.tt("dve", h0t[:], h0t[:], gbi[:, 1, :], ALU.add)
                    S.dma("qsp", A["h0own_d"][t * 128 : (t + 1) * 128, :], h0t[:])


def phase1(S, nc, A, P):
    from contextlib import ExitStack
    import os
    KP1 = int(os.environ.get("KP1", "99"))
    NBLK = int(os.environ.get("KBLK", "8"))

    identb, identf, UIf, SLf, ILf, onesb, onesf = P["identb"], P["identf"], P["UIf"], P["SLf"], P["ILf"], P["onesb"], P["onesf"]
    with ExitStack() as st:
        sb = lambda n, s, d: st.enter_context(nc.sbuf_tensor(n, s, d))
        NC_ = NCHK * NH
        beta = sb("g_beta", [128, NC_], F32)
        nbeta = sb("g_nbeta", [128, NC_], F32)
        bg = sb("g_bg", [128, NC_], F32)
        eg = sb("g_eg", [128, NC_], F32)
        egl = sb("g_egl", [128, NC_], F32)
        ekd = sb("g_ekd", [128, NC_], F32)
        gstep = sb("g_step", [128, NC_], F32)
        gtmp = sb("g_tmp", [128, NC_], F32)
        ba = P["ba_tok"]
        v3 = lambda t: t[:].rearrange("p (n h) -> p n h", h=NH)
        S.act(v3(beta), ba[:, :, 0:32], AF.Sigmoid)
        S.ts("dve", nbeta[:], beta[:], -1.0, None, ALU.mult)
        S.tt("dve", v3(gtmp), ba[:, :, 32:64], P["gpar"][:, 1, :].rearrange("p (n h) -> p n h", h=NH), ALU.add)
        S.act(gtmp[:], gtmp[:], AF.Exp)
        S.act(gtmp[:], gtmp[:], AF.Ln, bias=1.0)
        S.act(gstep[:], P["gpar"][:, 0, :], AF.Exp)
        S.stt("dve", gstep[:], gtmp[:], -1.0, gstep[:], ALU.mult, ALU.mult)
        with ExitStack() as st2:
            pg = [st2.enter_context(nc.psum_tensor("pgs%d" % i, [128, 512], F32)) for i in range(4)]
            for hlf in range(2):
                cs = slice(hlf * 512, (hlf + 1) * 512)
                for q4 in range(4):
                    c4 = slice(hlf * 512 + q4 * 128, hlf * 512 + (q4 + 1) * 128)
                    S.mm(pg[hlf][:, q4 * 128 : (q4 + 1) * 128], UIf[:], gstep[:, c4])
                    S.mm(pg[2 + hlf][:, q4 * 128 : (q4 + 1) * 128], onesf[:], gstep[:, c4])
                S.copy("dve", gtmp[:, cs], pg[hlf][:])
                S.act(eg[:, cs], pg[hlf][:], AF.Exp)
                S.act(egl[:, cs], pg[2 + hlf][:], AF.Exp)
                S.tt("dve", ekd[:, cs], pg[2 + hlf][:], gtmp[:, cs], ALU.subtract)
                S.act(ekd[:, cs], ekd[:, cs], AF.Exp)
            S.tt("dve", bg[:], beta[:], eg[:], ALU.mult)
        S.barrier()

        w_pool = Pool(st, nc, "gw", [128, KC, 512], BF16, 1)
        hb_pool = Pool(st, nc, "ghb", [128, KC, 512], BF16, 2)
        xpre = [sb("xpre%d" % i, [128, 515], F32) for i in range(4)]
        cacc = Pool(st, nc, "cacc", [128, 512], F32, 3)
        xsf = Pool(st, nc, "xsf", [128, 512], F32, 3)
        vsb = Pool(st, nc, "vsb", [128, 512], BF16, 4)
        sqb = Pool(st, nc, "sqb", [128, 512], BF16, 2)
        rsp = Pool(st, nc, "rsp", [128, 512], F32, 2)
        qkn = Pool(st, nc, "qkn", [128, 512], BF16, 4)
        NCHAIN = 8
        clong, cshort, ckk = [], [], []
        for ch in range(NCHAIN):
            tb = sb("clb%d" % ch, [128, 6, 128], BF16)
            tf = sb("clf%d" % ch, [128, 3, 128], F32)
            clong.append({"attnT": tb[:, 0, :], "wT": tb[:, 1, :], "qd": tb[:, 2, :], "kbg": tb[:, 3, :], "kdec": tb[:, 4, :], "vb": tb[:, 5, :],
                          "Gm": tf[:, 0, :], "Dm": tf[:, 1, :], "u": tf[:, 2, :]})
            cshort.append(Slots(st, nc, "csh%d_" % ch, BF16, 1, 9, psum=False))
        for c in range(4):
            tk = sb("ckk%d" % c, [128, 2, 128], F32)
            ckk.append((tk[:, 0, :], tk[:, 1, :]))
        cvn = [Slots(st, nc, "cvn%d_" % hh, BF16, 1, 2, psum=False) for hh in range(2)]
        Sf = sb("Sf", [128, 2, 128], F32)
        Sb = sb("Sb", [128, 2, 128], BF16)
        oacc_pool = Pool(st, nc, "oacc", [128, 8, 2, 128], F32, 1)
        pproj = Pool(st, nc, "pproj", [128, 512], F32, 2, psum=True)
        ptb = Slots(st, nc, "ptb", BF16, 1, 8)
        psm = Slots(st, nc, "psm", F32, 5, 4)
        cw = P["cw"]
        h0T_v = A["h0T_d"].rearrange("c p t -> p c t")
        o_v = A["o_own_d"].rearrange("(l p) (h d) -> p l h d", p=128, d=128)

        for g in range(GDN_GROUPS if KP1 >= 2 else 0):
            wt = w_pool.get()
            S.dma("qpool", wt[:, :, 0:128], A["w_in_v"][:, :, C_Q + 128 * g : C_Q + 128 * (g + 1)])
            S.dma("qpool", wt[:, :, 128:256], A["w_in_v"][:, :, C_K + 128 * g : C_K + 128 * (g + 1)])
            S.dma("qpool", wt[:, :, 256:512], A["w_in_v"][:, :, C_V + 256 * g : C_V + 256 * (g + 1)])
            for ci in range(4):
                S.memset("dve", xpre[ci][:, 0:3], 0.0)
            S.memset("dve", Sf[:], 0.0)
            S.memset("dve", Sb[:], 0.0)
            oacc = oacc_pool.get()
            S.memset("dve", oacc[:], 0.0)
            cwi = [g, 16 + g, 32 + 2 * g, 33 + 2 * g]
            resA = {}

            def stageA(b):
                hb = hb_pool.get()
                S.dma("qsp", hb[:], h0T_v[:, :, b * 512 : (b + 1) * 512])
                outs = []
                for ci in range(4):
                    pp = pproj.get()
                    for c in range(KC):
                        S.mm(pp[:], wt[:, c, ci * 128 : (ci + 1) * 128], hb[:, c, :], start=(c == 0), stop=(c == KC - 1))
                    xp = xpre[ci]
                    S.copy("act", xp[:, 3:515], pp[:])
                    yield
                    acc = cacc.get()
                    S.ts("dve", acc[:], xp[:, 3:515], cw[:, cwi[ci], 3:4], None, ALU.mult)
                    for i in range(3):
                        S.stt("dve", acc[:], xp[:, i : i + 512], cw[:, cwi[ci], i : i + 1], acc[:], ALU.mult, ALU.add)
                        yield
                    S.copy("dve", xp[:, 0:3], xp[:, 512:515])
                    if ci < 2:
                        xs = xsf.get()
                        S.act(xs[:], acc[:], AF.Silu)
                        sq = sqb.get()
                        S.act(sq[:], xs[:], AF.Square)
                        yield
                        pss = pproj.get()
                        S.mm(pss[:], onesb[:], sq[:])
                        rs = rsp.get()
                        if ci == 0:
                            S.act(rs[:], pss[:], AF.Sqrt, bias=128.0 * RMS_EPS, scale=128.0)
                        else:
                            S.act(rs[:], pss[:], AF.Sqrt, bias=RMS_EPS, scale=1.0)
                        yield
                        S.add("dve", lambda e, rs=rs: e.reciprocal(rs[:], rs[:]), reads=[rs[:]], writes=[rs[:]])
                        xn = qkn.get()
                        S.tt("dve", xn[:], xs[:], rs[:], ALU.mult)
                        outs.append(xn)
                    else:
                        vs = vsb.get()
                        S.act(vs[:], acc[:], AF.Silu)
                        outs.append(vs)
                    yield
                resA[b] = outs

            for _ in stageA(0):
                pass
            for blk in range(NBLK):
                outs = resA[blk]
                qn, kn, vs0, vs1 = outs
                vss = (vs0, vs1)
                ctx = {}
                for c in range(4):
                    n = blk * 4 + c
                    csl = slice(c * 128, (c + 1) * 128)
                    kc_ = kn[:, csl]
                    qc_ = qn[:, csl]
                    pKK = psm.get()
                    S.mm(pKK, kc_, kc_)
                    KKm = ckk[c][0]
                    S.tt("dve", KKm, pKK, SLf[:], ALU.mult)
                    pQK = psm.get()
                    S.mm(pQK, qc_, kc_)
                    QKm = ckk[c][1]
                    S.tt("dve", QKm, pQK, ILf[:], ALU.mult)
                    pkt = ptb.get()
                    S.tr(pkt, kc_, identb[:])
                    for hh in range(2):
                        col = n * NH + 2 * g + hh
                        L = clong[c * 2 + hh]
                        S.act(L["kbg"], pkt, AF.Identity, scale=bg[:, col : col + 1])
                        S.act(L["kdec"], pkt, AF.Identity, scale=ekd[:, col : col + 1])
                    for hh in range(2):
                        col = n * NH + 2 * g + hh
                        L = clong[c * 2 + hh]
                        pvt = ptb.get()
                        S.tr(pvt, vss[hh][:, csl], identb[:])
                        S.act(L["vb"], pvt, AF.Identity, scale=beta[:, col : col + 1])

                def pre_chain(c, hh):
                    n = blk * 4 + c
                    csl = slice(c * 128, (c + 1) * 128)
                    qc_ = qn[:, csl]
                    col = n * NH + 2 * g + hh
                    cs1 = slice(col, col + 1)
                    ch = c * 2 + hh
                    L = clong[ch]
                    sh = cshort[ch]
                    KKm, QKm = ckk[c]
                    Gm, Dm = L["Gm"], L["Dm"]
                    S.ts("dve", Gm, UIf[:], gstep[:, cs1], None, ALU.mult)
                    yield
                    pD = psm.get()
                    S.mm(pD, Gm, SLf[:])
                    S.act(Dm, pD, AF.Exp)
                    yield
                    N = sh.get()
                    S.stt("dve", N, KKm, nbeta[:, cs1], Dm, ALU.mult, ALU.mult)
                    At = sh.get()
                    S.tt("dve", At, QKm, Dm, ALU.mult)
                    yield
                    pNT = ptb.get()
                    S.tr(pNT, N, identb[:])
                    NT = sh.get()
                    S.copy("act", NT, pNT)
                    PT = sh.get()
                    S.tt("dve", PT, pNT, identb[:], ALU.add)
                    pAT = ptb.get()
                    S.tr(pAT, At, identb[:])
                    S.copy("act", L["attnT"], pAT)
                    yield
                    Am, AT = N, NT
                    for lvl in range(6):
                        pA2 = psm.get()
                        S.mm(pA2, AT, Am)
                        A2 = sh.get()
                        S.copy("act", A2, pA2)
                        A2T = None
                        if lvl < 5:
                            pA2T = psm.get()
                            S.mm(pA2T, Am, AT)
                            A2T = sh.get()
                            S.copy("act" if lvl % 2 else "dve", A2T, pA2T)
                        yield
                        pP = psm.get()
                        S.mm(pP, A2, PT)
                        PTn = sh.get()
                        S.tt("dve", PTn, pP, PT, ALU.add)
                        PT = PTn
                        Am, AT = A2, A2T
                        yield
                    pu = psm.get()
                    S.mm(pu, PT, L["vb"])
                    S.copy("act", L["u"], pu)
                    pw = psm.get()
                    S.mm(pw, L["kbg"], PT)
                    S.copy("act", L["wT"], pw)
                    dg = sh.get()
                    S.ts("dve", dg, identb[:], eg[:, cs1], None, ALU.mult)
                    yield
                    pe_ = psm.get()
                    S.mm(pe_, onesb[:], dg)
                    S.tt("dve", L["qd"], pe_, qc_, ALU.mult)
                    yield

                def scan_chain(c, hh):
                    n = blk * 4 + c
                    col = n * NH + 2 * g + hh
                    cs1 = slice(col, col + 1)
                    L = clong[c * 2 + hh]
                    pws = psm.get()
                    S.mm(pws, L["wT"], Sb[:, hh, :])
                    vn = cvn[hh].get()
                    S.tt("dve", vn, L["u"], pws, ALU.subtract)
                    yield
                    po = psm.get()
                    S.mm(po, L["qd"], Sb[:, hh, :], start=True, stop=False)
                    S.mm(po, L["attnT"], vn, start=False, stop=True)
                    pdS = psm.get()
                    S.mm(pdS, L["kdec"], vn)
                    yield
                    oa = oacc[:, n % 8, hh, :]
                    S.stt("dve", oa, po, P["mseg"][:, n // 8 : n // 8 + 1], oa, ALU.mult, ALU.add)
                    S.stt("dve", Sf[:, hh, :], Sf[:, hh, :], egl[:, cs1], pdS, ALU.mult, ALU.add)
                    S.copy("act", Sb[:, hh, :], Sf[:, hh, :])
                    yield

                def run_interleaved(gens):
                    gens = list(gens)
                    while gens:
                        nxt = []
                        for gg in gens:
                            try:
                                next(gg)
                                nxt.append(gg)
                            except StopIteration:
                                pass
                        gens = nxt

                gens_ = [pre_chain(c, hh) for c in range(4) for hh in range(2)]
                if blk + 1 < NBLK:
                    gens_.append(stageA(blk + 1))
                run_interleaved(gens_)
                for c in range(4):
                    run_interleaved([scan_chain(c, hh) for hh in range(2)])
            S.dma("qsp", o_v[:, :, 2 * g : 2 * g + 2, :], oacc[:])


def phase2(S, nc, A, P):
    from contextlib import ExitStack

    identb, identf = P["identb"], P["identf"]
    wiv = A["w_in_v"]
    with ExitStack() as st:
        bank = Pool(st, nc, "pb2", [128, 512], F32, 6, psum=True)
        ptr = Pool(st, nc, "ptr2", [128, 8, 128], BF16, 1, psum=True)
        ptf = Slots(st, nc, "ptf2", F32, 1, 4)
        for hf in range(2):
            t0 = 128 + hf * 512
            with ExitStack() as sh:
                sbh = lambda n, s, d: sh.enter_context(nc.sbuf_tensor("%s_h%d" % (n, hf), s, d))
                h0w = sbh("h0w", [128, KC, 640], BF16)
                S.dma("qsp", h0w[:], A["h0Te_d"].rearrange("c p t -> p c t")[:, :, t0 - 128 : t0 + 512])
                onT = sbh("onT", [128, 32, 512], BF16)
                pT = sbh("pT", [128, KC, 512], BF16)
                mT = sbh("mT", [128, KC, 512], BF16)
                hw_own = lambda c: h0w[:, c, 128:640]
                with ExitStack() as sa:
                    nm = lambda n: "%s_a%d" % (n, hf)
                    on0 = [sa.enter_context(nc.sbuf_tensor(nm("on0_%d" % i), [128, 4096], BF16)) for i in range(4)]
                    big = Pool(sa, nc, nm("big"), [128, 4096], F32, 1)
                    ss_pool = Pool(sa, nc, nm("ss"), [128, 32], F32, 2)
                    wpool = Pool(sa, nc, nm("wz"), [128, KC, 512], BF16, 2)
                    f512 = Pool(sa, nc, nm("f5"), [128, 512], F32, 4)
                    for tt_ in range(4):
                        tile = hf * 4 + tt_
                        ot = big.get()
                        S.dma("qsp", ot[:], A["o_own_d"][tile * 128 : (tile + 1) * 128, :])
                        S.act(on0[tt_][:], ot[:], AF.Square)
                        ss = ss_pool.get()
                        S.add("dve", lambda e, ss=ss, sq=on0[tt_]: e.reduce_sum(ss[:], sq[:].rearrange("p (h d) -> p h d", d=128), AX.X), reads=[on0[tt_][:]], writes=[ss[:]])
                        S.act(ss[:], ss[:], AF.Sqrt, bias=RMS_EPS, scale=1.0 / 128.0)
                        S.add("dve", lambda e, ss=ss: e.reciprocal(ss[:], ss[:]), reads=[ss[:]], writes=[ss[:]])
                        for h in range(NH):
                            S.stt("dve", on0[tt_][:, h * 128 : (h + 1) * 128], ot[:, h * 128 : (h + 1) * 128], ss[:, h : h + 1], P["wn_bc"][:], ALU.mult, ALU.mult)
                    for zb in range(8):
                        wz = wpool.get()
                        S.dma("qpool", wz[:], wiv[:, :, C_Z + zb * 512 : C_Z + (zb + 1) * 512])
                        for tt_ in range(4):
                            pz = bank.get()
                            cb = 128 + tt_ * 128
                            for c in range(KC):
                                S.mm(pz[:], h0w[:, c, cb : cb + 128], wz[:, c, :], start=(c == 0), stop=(c == KC - 1))
                            sz = f512.get()
                            S.act(sz[:], pz[:], AF.Silu)
                            dst = on0[tt_][:, zb * 512 : (zb + 1) * 512]
                            S.tt("dve", dst, dst, sz[:], ALU.mult)
                    for tt_ in range(4):
                        for h8 in range(4):
                            pt = ptr.get()
                            for c in range(8):
                                hc = h8 * 8 + c
                                S.tr(pt[:, c, :], on0[tt_][:, hc * 128 : (hc + 1) * 128], identb[:])
                            S.copy("act", onT[:, h8 * 8 : (h8 + 1) * 8, tt_ * 128 : (tt_ + 1) * 128], pt[:])
                S.barrier()
                with ExitStack() as sa:
                    nm = lambda n: "%s_b%d" % (n, hf)
                    wpool = Pool(sa, nc, nm("wsc"), [128, KC, 384], BF16, 2)
                    f512 = Pool(sa, nc, nm("f5"), [128, 512], F32, 4)
                    u_pool = Pool(sa, nc, nm("u"), [128, 514], F32, 2)
                    hs_pool = Pool(sa, nc, nm("hs"), [128, 4], F32, 2)
                    for fc in range(KC):
                        wsc = wpool.get()
                        for k3, cbase in enumerate((C_SB, C_SC, C_SX)):
                            S.dma("qpool", wsc[:, :, k3 * 128 : (k3 + 1) * 128], wiv[:, :, cbase + fc * 128 : cbase + (fc + 1) * 128])
                        pc, px, pb_ = bank.get(), bank.get(), bank.get()
                        ph = ptf.get()
                        for c in range(KC):
                            S.mm(pc[:], wsc[:, c, 128:256], hw_own(c), start=(c == 0), stop=(c == KC - 1))
                        for c in range(KC):
                            S.mm(px[:], wsc[:, c, 256:384], hw_own(c), start=(c == 0), stop=(c == KC - 1))
                        for c in range(KC):
                            S.mm(pb_[:], wsc[:, c, 0:128], hw_own(c), start=(c == 0), stop=(c == KC - 1))
                        for c in range(KC):
                            S.mm(ph[:, 0:2], wsc[:, c, 128:256], h0w[:, c, 126:128], start=(c == 0), stop=(c == KC - 1))
                        for c in range(KC):
                            S.mm(ph[:, 2:4], wsc[:, c, 256:384], h0w[:, c, 126:128], start=(c == 0), stop=(c == KC - 1))
                        ccs = f512.get()
                        S.copy("act", ccs[:], pc[:])
                        u = u_pool.get()
                        S.tt("dve", u[:, 2:514], px[:], ccs[:], ALU.mult)
                        hs = hs_pool.get()
                        S.copy("act", hs[:], ph[:, 0:4])
                        S.tt("dve", u[:, 0:2], hs[:, 0:2], hs[:, 2:4], ALU.mult)
                        if hf == 0:
                            S.tt("dve", u[:, 0:2], u[:, 0:2], P["mh"][:], ALU.mult)
                        acc = f512.get()
                        scw = P["scw"]
                        S.ts("dve", acc[:], u[:, 2:514], scw[:, fc, 2:3], None, ALU.mult)
                        S.stt("dve", acc[:], u[:, 1:513], scw[:, fc, 1:2], acc[:], ALU.mult, ALU.add)
                        S.stt("dve", acc[:], u[:, 0:512], scw[:, fc, 0:1], acc[:], ALU.mult, ALU.add)
                        S.tt("dve", pT[:, fc, :], pb_[:], acc[:], ALU.mult)
                S.barrier()
                with ExitStack() as sa:
                    nm = lambda n: "%s_c%d" % (n, hf)
                    wm_pool = Pool(sa, nc, nm("wm"), [128, 80, 128], BF16, 2)
                    f512 = Pool(sa, nc, nm("f5"), [128, 512], F32, 4)
                    for fc in range(KC):
                        wm = wm_pool.get()
                        fs = slice(fc * 128, (fc + 1) * 128)
                        S.dma("qpool", wm[:, 0:32, :], A["w_out_gdn"].rearrange("(c p) n -> p c n", p=128)[:, :, fs])
                        S.dma("qpool", wm[:, 32:48, :], A["w_out_sc"].rearrange("(c p) n -> p c n", p=128)[:, :, fs])
                        S.dma("qpool", wm[:, 48:64, :], wiv[:, :, C_GA + fc * 128 : C_GA + (fc + 1) * 128])
                        S.dma("qpool", wm[:, 64:80, :], wiv[:, :, C_GB + fc * 128 : C_GB + (fc + 1) * 128])
                        pya, pga, pyb, pgb = bank.get(), bank.get(), bank.get(), bank.get()
                        for c in range(32):
                            S.mm(pya[:], wm[:, c, :], onT[:, c, :], start=(c == 0), stop=(c == 31))
                        for c in range(KC):
                            S.mm(pga[:], wm[:, 48 + c, :], hw_own(c), start=(c == 0), stop=(c == KC - 1))
                        for c in range(KC):
                            S.mm(pyb[:], wm[:, 32 + c, :], pT[:, c, :], start=(c == 0), stop=(c == KC - 1))
                        for c in range(KC):
                            S.mm(pgb[:], wm[:, 64 + c, :], hw_own(c), start=(c == 0), stop=(c == KC - 1))
                        sga, sgb = f512.get(), f512.get()
                        S.act(sga[:], pga[:], AF.Sigmoid)
                        S.tt("dve", sga[:], pya[:], sga[:], ALU.mult)
                        S.act(sgb[:], pgb[:], AF.Sigmoid)
                        S.tt("dve", sgb[:], pyb[:], sgb[:], ALU.mult)
                        S.tt("dve", mT[:, fc, :], sga[:], sgb[:], ALU.add)
                S.barrier()
                with ExitStack() as sa:
                    nm = lambda n: "%s_d%d" % (n, hf)
                    wpool = Pool(sa, nc, nm("wo"), [128, KC, 512], BF16, 2)
                    h1t = sa.enter_context(nc.sbuf_tensor(nm("h1t"), [128, 4, D], F32))
                    gbm = sa.enter_context(nc.sbuf_tensor(nm("gbm"), [128, 2, D], F32))
                    S.dma("qsp", gbm[:], A["gb_mix"])
                    lsq = Pool(sa, nc, nm("lsq"), [128, D], F32, 1)
                    st_pool = Pool(sa, nc, nm("lst"), [128, 8], F32, 3)
                    tmpT_pool = Pool(sa, nc, nm("tmpT"), [128, KC, 128], F32, 1)
                    hTs_pool = Pool(sa, nc, nm("hTs"), [128, KC, 128], BF16, 2)
                    lg_pool = Pool(sa, nc, nm("lg"), [128, NE], F32, 2)
                    for tt_ in range(4):
                        tile = hf * 4 + tt_
                        S.dma("qsp", h1t[:, tt_, :], A["h0own_d"][128 + tile * 128 : 128 + (tile + 1) * 128, :])
                    for cb in range(4):
                        wo = wpool.get()
                        S.dma("qpool", wo[:], A["w_out"].rearrange("(c p) n -> p c n", p=128)[:, :, cb * 512 : (cb + 1) * 512])
                        for tt_ in range(4):
                            pm = bank.get()
                            for c in range(KC):
                                S.mm(pm[:], mT[:, c, tt_ * 128 : (tt_ + 1) * 128], wo[:, c, :], start=(c == 0), stop=(c == KC - 1))
                            dst = h1t[:, tt_, cb * 512 : (cb + 1) * 512]
                            S.stt("dve", dst, dst, ALPHA, pm[:], ALU.mult, ALU.add)
                    for tt_ in range(4):
                        tile = hf * 4 + tt_
                        hv = h1t[:, tt_, :]
                        rstd, nmr = ln_stats(S, hv, D, st_pool, lsq)
                        S.act(hv, hv, AF.Identity, bias=nmr, scale=rstd)
                        S.tt("dve", hv, hv, gbm[:, 0, :], ALU.mult)
                        S.tt("dve", hv, hv, gbm[:, 1, :], ALU.add)
                        S.dma("qsp", A["h1_d"][tile * 128 : (tile + 1) * 128, :], hv)
                        tmpT = tmpT_pool.get()
                        hTs = hTs_pool.get()
                        for c in range(KC):
                            pt = ptf.get()
                            S.tr(pt, h1t[:, tt_, c * 128 : (c + 1) * 128], identf[:])
                            S.copy("act", hTs[:, c, :], pt)
                            S.copy("dve", tmpT[:, c, :], pt)
                        S.dma("qsp", A["h1T_d"].rearrange("c p t -> p c t")[:, :, tile * 128 : (tile + 1) * 128], hTs[:])
                        plg = ptf.get()
                        for c in range(KC):
                            S.mm(plg[:, 0:NE], tmpT[:, c, :], P["wr"][:, c, :], start=(c == 0), stop=(c == KC - 1))
                        lg = lg_pool.get()
                        S.tt("dve", lg[:], plg[:, 0:NE], P["br_bc"][:], ALU.add)
                        S.dma("qsp", A["lg_d"][tile * 128 : (tile + 1) * 128, :], lg[:])
                S.barrier()
        S.barrier()


def phase3(S, nc, A, P):
    from contextlib import ExitStack

    identf = P["identf"]
    with ExitStack() as st:
        sb = lambda n, s, d: st.enter_context(nc.sbuf_tensor(n, s, d))
        h1 = sb("h1", [128, 8, D], F32)
        h1T = sb("h1T", [128, KC, OWN], BF16)
        logits = sb("logits", [128, 8, NE], F32)
        S.dma("qsp", h1[:], A["h1_d"].rearrange("(t p) d -> p t d", p=128))
        S.dma("qsp", h1T[:], A["h1T_d"].rearrange("c p t -> p c t"))
        S.dma("qsp", logits[:], A["lg_d"].rearrange("(t p) e -> p t e", p=128))
        G = sb("G", [128, 8, NE], F32)
        st_outer = st
        st = st_outer.enter_context(ExitStack())
        sb = lambda n, s, d: st.enter_context(nc.sbuf_tensor(n, s, d))
        GT = sb("GT", [NE, 8, 128], F32)
        bd = sb("bd", [NE, D], F32)
        S.dma("qsp", bd[:], A["b_down"])
        bgu = sb("bgu_sb", [128, NE, KC, 2], F32)
        S.dma("qsp", bgu[:], A["bgu"])
        m8 = sb("m8", [128, 8, 8], F32)
        sm = sb("rt_sm", [128, 8, 4], F32)
        em = sb("rt_em", [128, 8, NE], F32)
        mk = sb("rt_mk", [128, 8, NE], F32)
        bank = Pool(st, nc, "pb3", [128, 512], F32, 7, psum=True)
        ptf = Slots(st, nc, "ptf3", F32, 1, 4)
        wpool = Pool(st, nc, "w3", [128, KC, 512], BF16, 2)
        actT = sb("actT", [128, KC, OWN], BF16)
        f512 = Pool(st, nc, "g512", [128, 512], F32, 4)
        for t in range(8):
            lg = logits[:, t, :]
            S.add("dve", lambda e, t=t, lg=lg: e.max(m8[:, t, :], lg), reads=[lg], writes=[m8[:, t, :]])
            S.ts("dve", mk[:, t, :], lg, m8[:, t, 3:4], None, ALU.is_ge)
            S.ts("dve", sm[:, t, 0:1], m8[:, t, 0:1], -1.0, None, ALU.mult)
            S.act(em[:, t, :], lg, AF.Exp, bias=sm[:, t, 0:1], scale=1.0)
            S.tt("dve", em[:, t, :], em[:, t, :], mk[:, t, :], ALU.mult)
            S.add("dve", lambda e, t=t: e.reduce_sum(sm[:, t, 1:2], em[:, t, :], AX.X), reads=[em[:, t, :]], writes=[sm[:, t, 1:2]])
            S.add("dve", lambda e, t=t: e.reciprocal(sm[:, t, 2:3], sm[:, t, 1:2]), reads=[sm[:, t, 1:2]], writes=[sm[:, t, 2:3]])
            S.ts("dve", G[:, t, :], em[:, t, :], sm[:, t, 2:3], None, ALU.mult)
            pt = ptf.get()
            S.tr(pt[0:NE, :], G[:, t, :], identf[:])
            S.copy("act", GT[:, t, :], pt[0:NE, :])
            for cb in range(4):
                pb = bank.get()
                for q4 in range(4):
                    S.mm(pb[:, q4 * 128 : (q4 + 1) * 128], GT[:, t, :], bd[:, cb * 512 + q4 * 128 : cb * 512 + (q4 + 1) * 128])
                dst = h1[:, t, cb * 512 : (cb + 1) * 512]
                S.stt("dve", dst, dst, ALPHA, pb[:], ALU.mult, ALU.add)
        for e in range(NE):
            wgu_v = A["w_gate_up"][e].rearrange("(c p) n -> p c n", p=128)
            wd_v = A["w_down"][e].rearrange("(c p) n -> p c n", p=128)
            for gb in range(8):
                wg = wpool.get()
                S.dma("qpool", wg[:], wgu_v[:, :, gb * 512 : (gb + 1) * 512])
                for sub in range(2):
                    fc = gb * 2 + sub
                    for th in range(2):
                        ts_ = slice(th * 512, (th + 1) * 512)
                        pg, pu = bank.get(), bank.get()
                        for c in range(KC):
                            S.mm(pg[:], wg[:, c, sub * 256 : sub * 256 + 256 : 2], h1T[:, c, ts_], start=(c == 0), stop=(c == KC - 1))
                        for c in range(KC):
                            S.mm(pu[:], wg[:, c, sub * 256 + 1 : sub * 256 + 256 : 2], h1T[:, c, ts_], start=(c == 0), stop=(c == KC - 1))
                        gt = f512.get()
                        S.ts("dve", gt[:], pg[:], bgu[:, e, fc, 0:1], 7.0, ALU.add, ALU.min)
                        sg = f512.get()
                        S.act(sg[:], gt[:], AF.Sigmoid, scale=1.702)
                        up = f512.get()
                        S.ts("dve", up[:], pu[:], bgu[:, e, fc, 1:2], 7.0, ALU.add, ALU.min)
                        S.ts("dve", up[:], up[:], -7.0, 1.0, ALU.max, ALU.add)
                        S.tt("dve", gt[:], gt[:], sg[:], ALU.mult)
                        S.tt("dve", actT[:, fc, ts_], gt[:], up[:], ALU.mult)
            for cb in range(4):
                wd = wpool.get()
                S.dma("qpool", wd[:], wd_v[:, :, cb * 512 : (cb + 1) * 512])
                for t in range(8):
                    pd = bank.get()
                    for c in range(KC):
                        S.mm(pd[:], actT[:, c, t * 128 : (t + 1) * 128], wd[:, c, :], start=(c == 0), stop=(c == KC - 1))
                    dst = h1[:, t, cb * 512 : (cb + 1) * 512]
                    S.stt("dve", dst, pd[:], G[:, t, e : e + 1], dst, ALU.mult, ALU.add)
        S.barrier()
        st.close()
        st = st_outer
        sb = lambda n, s, d: st.enter_context(nc.sbuf_tensor(n, s, d))
        gbf = sb("gbf", [128, 2, D], F32)
        S.dma("qsp", gbf[:], A["gb_ffn"])
        st_pool = Pool(st, nc, "lst3", [128, 8], F32, 3)
        sq_pool = Pool(st, nc, "sq3", [128, D], F32, 1)
        o_pool = Pool(st, nc, "o3", [128, D], F32, 2)
        for t in range(8):
            hv = h1[:, t, :]
            rstd, nmr = ln_stats(S, hv, D, st_pool, sq_pool)
            ot = o_pool.get()
            S.act(ot[:], hv, AF.Identity, bias=nmr, scale=rstd)
            S.tt("dve", ot[:], ot[:], gbf[:, 0, :], ALU.mult)
            S.tt("dve", ot[:], ot[:], gbf[:, 1, :], ALU.add)
            S.dma("qsp", A["out"][t * 128 : (t + 1) * 128, :], ot[:])


def build_program():
    from contextlib import ExitStack

    nc = bass.Bass("TRN2", target_bir_lowering=False)
    A = {}

    def din(name, shape):
        A[name] = nc.dram_tensor(name, shape, F32, kind="ExternalInput").ap()
        return A[name]

    din("xb", [SEQ, D])
    din("xe", [EXT, D])
    din("consts", [128, 5, 128])
    din("lnin", [128, 2, KC])
    din("gb_in", [128, 2, D])
    din("gb_mix", [128, 2, D])
    din("gb_ffn", [128, 2, D])
    din("gpar", [128, 2, NCHK * NH])
    din("cw", [128, 64, 4])
    din("scw", [128, KC, 3])
    din("wn_bc", [128, 128])
    din("mseg", [128, 4])
    din("mh", [128, 2])
    din("wr", [128, KC, NE])
    din("br_bc", [128, NE])
    import os
    NOW = os.environ.get("KNOW", "0") == "1"
    if not NOW:
        din("w_in", [D, 22592])
        din("w_out_gdn", [4096, D])
        din("w_out_sc", [D, D])
        din("w_out", [D, D])
    else:
        din("w_in", [128, 22592])
        A["w_in_v"] = bass.AP(A["w_in"].tensor, 0, [[22592, 128], [0, KC], [1, 22592]])
    if 3 in PHASES:
        din("w_gate_up", [NE, D, 4096])
        din("w_down", [NE, D, D])
    din("bgu", [128, NE, KC, 2])
    din("b_down", [NE, D])
    if not NOW:
        A["w_in_v"] = A["w_in"].rearrange("(c p) n -> p c n", p=128)
    A["out"] = nc.dram_tensor("out", [OWN, D], F32, kind="ExternalOutput").ap()
    kd = "ExternalOutput" if DEBUG else "Internal"
    A["h0T_d"] = nc.dram_tensor("h0T_d", [KC, 128, SEQ], BF16, kind=kd).ap()
    A["h0Te_d"] = nc.dram_tensor("h0Te_d", [KC, 128, EXT], BF16, kind=kd).ap()
    A["h0own_d"] = nc.dram_tensor("h0own_d", [EXT, D], F32, kind=kd).ap()
    A["o_own_d"] = nc.dram_tensor("o_own_d", [OWN, 4096], F32, kind=kd).ap()
    A["h1_d"] = nc.dram_tensor("h1_d", [OWN, D], F32, kind=kd).ap()
    A["h1T_d"] = nc.dram_tensor("h1T_d", [KC, 128, OWN], BF16, kind=kd).ap()
    A["lg_d"] = nc.dram_tensor("lg_d", [OWN, NE], F32, kind=kd).ap()

    S = Sched(nc)
    P = {}
    with ExitStack() as st:
        sb = lambda n, s, d: st.enter_context(nc.sbuf_tensor(n, s, d))
        cf = sb("cf", [128, 5, 128], F32)
        cb = sb("cb", [128, 5, 128], BF16)
        S.dma("qsp", cf[:], A["consts"])
        S.dma("qpool", cb[:], A["consts"])
        P["identf"], P["UIf"], P["SLf"], P["ILf"], P["onesf"] = (cf[:, i, :] for i in range(5))
        P["identb"], P["onesb"] = cb[:, 0, :], cb[:, 4, :]
        for name, shape in (("lnin", [128, 2, KC]), ("cw", [128, 64, 4]), ("scw", [128, KC, 3]), ("wn_bc", [128, 128]),
                            ("mseg", [128, 4]), ("mh", [128, 2]), ("wr", [128, KC, NE]), ("br_bc", [128, NE])):
            t = sb("c_" + name, shape, F32)
            S.dma("qsp", t[:], A[name])
            P[name] = t
        with ExitStack() as st01:
            P["ba_tok"] = st01.enter_context(nc.sbuf_tensor("ba_tok", [128, NCHK, 64], F32))
            P["gpar"] = st01.enter_context(nc.sbuf_tensor("gpar_sb", [128, 2, NCHK * NH], F32))
            S.dma("qsp", P["gpar"][:], A["gpar"])
            if 0 in PHASES:
                phase0(S, nc, A, P)
            S.barrier()
            if 1 in PHASES:
                phase1(S, nc, A, P)
            S.barrier()
        if 2 in PHASES:
            phase2(S, nc, A, P)
        S.barrier()
        if 3 in PHASES:
            phase3(S, nc, A, P)
        S.emit()
    return nc


def _prep_inputs(inp):
    f = lambda a: np.ascontiguousarray(a, dtype=np.float32)
    x = inp["x"]
    pc = lambda v: f(np.asarray(v).reshape(-1, 128).T)
    bc = lambda v: f(np.broadcast_to(np.asarray(v).reshape(1, -1), (128, np.asarray(v).size)))
    pidx = np.arange(128)[:, None]
    fidx = np.arange(128)[None, :]
    consts = np.stack([(pidx == fidx), (pidx <= fidx), (pidx > fidx), (pidx >= fidx), np.ones((128, 128), bool)], axis=1)
    common = {
        "consts": f(consts),
        "lnin": f(np.stack([pc(inp["ln_in_g"]), pc(inp["ln_in_b"])], axis=1)),
        "gb_in": f(np.stack([bc(inp["ln_in_g"]), bc(inp["ln_in_b"])], axis=1)),
        "gb_mix": f(np.stack([bc(inp["ln_mix_g"][0]), bc(inp["ln_mix_b"][0])], axis=1)),
        "gb_ffn": f(np.stack([bc(inp["ln_ffn_g"][0]), bc(inp["ln_ffn_b"][0])], axis=1)),
        "gpar": f(np.stack([bc(np.tile(inp["gdn_a_log"][0], NCHK)), bc(np.tile(inp["gdn_dt_bias"][0], NCHK))], axis=1)),
        "cw": f(np.asarray(inp["gdn_conv_w"][0]).T.reshape(64, 128, 4).transpose(1, 0, 2)),
        "scw": f(np.asarray(inp["sc_conv_w"][0]).T.reshape(KC, 128, 3).transpose(1, 0, 2)),
        "wn_bc": bc(inp["gdn_norm_w"][0]),
        "wr": f(np.asarray(inp["w_router"][0]).reshape(KC, 128, NE).transpose(1, 0, 2)),
        "br_bc": bc(inp["b_router"][0]),
        "w_in": f(inp["w_in"][0]),
        "w_out_gdn": f(inp["w_out_gdn"][0]),
        "w_out_sc": f(inp["w_out_sc"][0]),
        "w_out": f(inp["w_out"][0]),
        "w_gate_up": f(inp["w_gate_up"][0]),
        "w_down": f(inp["w_down"][0]),
        "bgu": f(np.asarray(inp["b_gate_up"][0]).reshape(NE, KC, 128, 2).transpose(2, 0, 1, 3)),
        "b_down": f(inp["b_down"][0]),
    }
    maps = []
    for c in range(8):
        b, j = c // 4, c % 4
        xe = np.zeros((EXT, D), np.float32)
        lo = OWN * j - 128
        if lo < 0:
            xe[128:] = x[b, 0:OWN]
        else:
            xe[:] = x[b, lo : lo + EXT]
        mseg = np.zeros((128, 4), np.float32)
        mseg[:, j] = 1.0
        mh = np.full((128, 2), 0.0 if j == 0 else 1.0, np.float32)
        m = dict(common)
        m.update({"xb": f(x[b]), "xe": xe, "mseg": mseg, "mh": mh})
        maps.append(m)
    return maps


_NC_CACHE = {}


def kernel(**inputs):
    inp = {k: np.asarray(v) for k, v in inputs.items()}
    if "nc" not in _NC_CACHE:
        _NC_CACHE["nc"] = build_program()
    nc = _NC_CACHE["nc"]
    maps = _prep_inputs(inp)
    res = run_bass_kernel_spmd(nc, maps, core_ids=list(range(8)))
    out = np.zeros((2, SEQ, D), np.float32)
    for c in range(8):
        b, j = c // 4, c % 4
        out[b, OWN * j : OWN * (j + 1)] = np.asarray(res.results[c]["out"], dtype=np.float32)
    if DEBUG:
        kernel.last = res
    return out
```

```python
import numpy as np
import concourse.bass as bass
import concourse.mybir as mybir
from concourse.bass_utils import run_bass_kernel_spmd

F32 = mybir.dt.float32
BF16 = mybir.dt.bfloat16
AF = mybir.ActivationFunctionType
ALU = mybir.AluOpType
AX = mybir.AxisListType

SEM_LIMIT = 8000
_ESZ = {F32: 4, BF16: 2, mybir.dt.int32: 4, mybir.dt.uint32: 4}


class _Op:
    __slots__ = ("eng", "fn", "deps", "idx", "sig", "semval", "dsem", "dval", "ring_wait")

    def __init__(self, eng, fn):
        self.eng = eng
        self.fn = fn
        self.deps = set()
        self.sig = False
        self.semval = 0
        self.dsem = None
        self.dval = 0
        self.ring_wait = None


def _region(ap):
    t = ap.tensor
    space = str(ap.space) if hasattr(ap, "space") else ""
    esz = _ESZ.get(ap.dtype, 4)
    dims = ap.ap
    if "DRAM" in space.upper() or "HBM" in space.upper() or type(t).__name__.startswith("DRam"):
        lo = ap.offset
        hi = lo + sum((c - 1) * s for s, c in dims)
        return (t.name, 0, 1, lo * esz, (hi + 1) * esz)
    if type(t).__name__.startswith("PSum"):
        return (t.name, 0, 128, 0, 1 << 30)
    pstep = dims[0][0]
    pcount = dims[0][1]
    rowel = 1
    for s in t.shape[1:]:
        rowel *= s
    tesz = _ESZ.get(t.dtype, 4)
    rowel = rowel * tesz // esz
    if pcount == 1 and pstep != rowel:
        pstep = rowel
    p0 = ap.offset // rowel
    f0 = ap.offset % rowel
    span = sum((c - 1) * s for s, c in dims[1:])
    return (t.name, p0, p0 + pcount, f0 * esz, (f0 + span + 1) * esz)


class Sched:
    COMPUTE = ("pe", "act", "dve", "pool")

    def __init__(self, nc, dma_ring=16):
        self.nc = nc
        self.ops = []
        self.by_eng = {}
        self.track = {}
        self.dma_ring = dma_ring
        self.dma_count = {}
        self.untracked = set()
        self.barrier_deps = set()
        self.barrier_id = 0
        self.eng_barrier = {}

    def add(self, eng, fn, reads=(), writes=()):
        op = _Op(eng, fn)
        op.idx = len(self.ops)
        self.ops.append(op)
        self.by_eng.setdefault(eng, []).append(op)
        for ap in reads:
            self._access(op, ap, False)
        for ap in writes:
            self._access(op, ap, True)
        hwk = {"qact": "act", "qpool": "pool"}.get(eng, eng)
        if self.eng_barrier.get(hwk, 0) < self.barrier_id:
            self.eng_barrier[hwk] = self.barrier_id
            op.deps |= {d for d in self.barrier_deps if d != op.idx}
        if eng.startswith("q"):
            n = self.dma_count.get(eng, 0)
            self.dma_count[eng] = n + 1
            op.dsem = n % self.dma_ring
            op.dval = 16 * (n // self.dma_ring + 1)
            if n >= self.dma_ring:
                op.ring_wait = (op.dsem, op.dval - 16)
        return op

    def barrier(self):
        deps = set()
        for e, lst in self.by_eng.items():
            if not lst:
                continue
            if e.startswith("q"):
                deps |= {o.idx for o in lst[-self.dma_ring:]}
            else:
                deps.add(lst[-1].idx)
        self.barrier_deps = deps
        self.barrier_id += 1
        self.track = {}

    def _access(self, op, ap, is_write):
        name, p0, p1, f0, f1 = _region(ap)
        if name in self.untracked:
            return
        if f1 == (1 << 30):
            is_write = True
        recs = self.track.setdefault(name, [])
        keep = []
        for r in recs:
            ov = not (r[1] <= p0 or p1 <= r[0] or r[3] <= f0 or f1 <= r[2])
            if not ov:
                keep.append(r)
                continue
            if r[4] != op.idx and (is_write or r[5]):
                op.deps.add(r[4])
            covered = p0 <= r[0] and r[1] <= p1 and f0 <= r[2] and r[3] <= f1
            if is_write and covered:
                continue
            if (not is_write) and (not r[5]) and covered and r[6] == op.eng and not op.eng.startswith("q"):
                continue
            keep.append(r)
        keep.append([p0, p1, f0, f1, op.idx, is_write, op.eng])
        self.track[name] = keep

    def emit(self):
        nc = self.nc
        ops = self.ops
        for op in ops:
            nd = set()
            for d in op.deps:
                dop = ops[d]
                if dop.eng == op.eng and op.eng == "pe":
                    continue
                nd.add(d)
                if not dop.eng.startswith("q"):
                    dop.sig = True
            op.deps = nd
        for e, lst in self.by_eng.items():
            if e.startswith("q"):
                continue
            c = 0
            for op in lst:
                if op.sig:
                    op.semval = (c // SEM_LIMIT, c % SEM_LIMIT + 1)
                    c += 1
            self.nsig = getattr(self, "nsig", {})
            self.nsig[e] = c
        from contextlib import ExitStack

        with ExitStack() as st:
            st.enter_context(nc.cleanup_on_exit())
            csem = {
                e: [nc.alloc_semaphore("s_%s_%d" % (e, i)) for i in range(self.nsig[e] // SEM_LIMIT + 1)]
                for e in self.by_eng
                if not e.startswith("q")
            }
            dsem = {
                e: [nc.alloc_semaphore("d_%s_%d" % (e, i)) for i in range(min(self.dma_ring, self.dma_count[e]))]
                for e in self.by_eng
                if e.startswith("q")
            }
            for lst_ in list(csem.values()) + list(dsem.values()):
                for s_ in lst_:
                    nc.gpsimd.sem_clear(s_)
            nc.all_engine_barrier()
            st.callback(nc.all_engine_barrier)
            block = st.enter_context(nc.Block())
            engmap = {
                "pe": "tensor",
                "act": "scalar",
                "dve": "vector",
                "pool": "gpsimd",
                "qsp": "sync",
                "qact": "scalar",
                "qpool": "gpsimd",
            }
            hw = {}
            for op in ops:
                hw.setdefault(engmap[op.eng], []).append(op)

            def make(hwname, lst):
                def body(eng):
                    waited = {}

                    def w(key, sem, val):
                        if waited.get(key, 0) >= val:
                            return
                        waited[key] = val
                        eng.wait_ge(sem, val)

                    for op in lst:
                        if op.ring_wait is not None:
                            w((op.eng, op.ring_wait[0]), dsem[op.eng][op.ring_wait[0]], op.ring_wait[1])
                        for d in sorted(op.deps):
                            dop = ops[d]
                            if dop.eng.startswith("q"):
                                w((dop.eng, dop.dsem), dsem[dop.eng][dop.dsem], dop.dval)
                            else:
                                w((dop.eng, dop.semval[0]), csem[dop.eng][dop.semval[0]], dop.semval[1])
                        ins = op.fn(eng)
                        if op.eng.startswith("q"):
                            ins.then_inc(dsem[op.eng][op.dsem], 16)
                        elif op.sig:
                            ins.then_inc(csem[op.eng][op.semval[0]], 1)
                    for qe in dsem:
                        if engmap[qe] != hwname:
                            continue
                        n = self.dma_count[qe]
                        for i, s in enumerate(dsem[qe]):
                            uses = (n - i + self.dma_ring - 1) // self.dma_ring
                            if uses > 0:
                                w((qe, i), s, 16 * uses)

                return body

            for hwname, lst in hw.items():
                getattr(block, hwname)(make(hwname, lst))

    def dma(self, q, out, in_, **kw):
        return self.add(q, lambda e: e.dma_start(out=out, in_=in_, **kw), reads=[in_], writes=[out])

    def mm(self, out, lhsT, rhs, start=True, stop=True):
        rd = [lhsT, rhs] + ([] if start else [out])
        return self.add("pe", lambda e: e.matmul(out, lhsT, rhs, start=start, stop=stop), reads=rd, writes=[out])

    def tr(self, out, in_, ident):
        return self.add("pe", lambda e: e.transpose(out, in_, ident), reads=[in_, ident], writes=[out])

    def act(self, out, in_, func, bias=None, scale=1.0, accum_out=None, eng="act"):
        rd = [in_]
        kw = {}
        if bias is not None:
            kw["bias"] = bias
            if not isinstance(bias, (int, float)):
                rd.append(bias)
        if not isinstance(scale, (int, float)):
            rd.append(scale)
        wr = [out]
        if accum_out is not None:
            kw["accum_out"] = accum_out
            wr.append(accum_out)
        return self.add(eng, lambda e: e.activation(out, in_, func, scale=scale, **kw), reads=rd, writes=wr)

    def tt(self, eng, out, in0, in1, op):
        return self.add(eng, lambda e: e.tensor_tensor(out, in0, in1, op), reads=[in0, in1], writes=[out])

    def ts(self, eng, out, in0, s1, s2, op0, op1=None, accum_out=None):
        rd = [in0] + [s for s in (s1, s2) if s is not None and not isinstance(s, (int, float))]
        wr = [out] + ([accum_out] if accum_out is not None else [])
        kw = {}
        if accum_out is not None:
            kw["accum_out"] = accum_out
        if op1 is None:
            return self.add(eng, lambda e: e.tensor_scalar(out, in0, s1, None, op0, **kw), reads=rd, writes=wr)
        return self.add(eng, lambda e: e.tensor_scalar(out, in0, s1, s2, op0, op1, **kw), reads=rd, writes=wr)

    def stt(self, eng, out, in0, scalar, in1, op0, op1):
        rd = [in0, in1] + ([] if isinstance(scalar, (int, float)) else [scalar])
        return self.add(eng, lambda e: e.scalar_tensor_tensor(out, in0, scalar, in1, op0, op1), reads=rd, writes=[out])

    def copy(self, eng, out, in_):
        if eng == "act":
            return self.add(eng, lambda e: e.copy(out, in_), reads=[in_], writes=[out])
        return self.add(eng, lambda e: e.tensor_copy(out, in_), reads=[in_], writes=[out])

    def memset(self, eng, ap, val):
        return self.add(eng, lambda e: e.memset(ap, val), reads=[], writes=[ap])


class Pool:
    def __init__(self, st, nc, name, shape, dtype, n, psum=False):
        mk = nc.psum_tensor if psum else nc.sbuf_tensor
        self.t = [st.enter_context(mk("%s%d" % (name, i), shape, dtype)) for i in range(n)]
        self.i = 0

    def get(self):
        t = self.t[self.i % len(self.t)]
        self.i += 1
        return t


D = 2048
KC = 16
SEQ = 4096
NCHK = 32
NH = 32
OWN = 1024
EXT = 1152
NE = 32
ALPHA = 2.0 ** 0.25
LN_EPS = 1e-5
RMS_EPS = 1e-6
C_Q, C_K, C_V, C_Z, C_B, C_A, C_SB, C_SC, C_SX, C_GA, C_GB = 0, 2048, 4096, 8192, 12288, 12320, 12352, 14400, 16448, 18496, 20544

DEBUG = False
PHASES = (0, 1, 2, 3)
GDN_GROUPS = 16


def ln_stats(S, xt, n, st_pool, sq_pool):
    stt = st_pool.get()
    sq = sq_pool.get()
    S.add("dve", lambda e: e.reduce_sum(stt[:, 0:1], xt, AX.X), reads=[xt], writes=[stt[:, 0:1]])
    sqv = sq[:, 0:n]
    S.act(sqv, xt, AF.Square)
    S.add("dve", lambda e: e.reduce_sum(stt[:, 1:2], sqv, AX.X), reads=[sqv], writes=[stt[:, 1:2]])
    S.ts("dve", stt[:, 2:3], stt[:, 0:1], 1.0 / n, None, ALU.mult)
    S.tt("dve", stt[:, 3:4], stt[:, 2:3], stt[:, 2:3], ALU.mult)
    S.stt("dve", stt[:, 4:5], stt[:, 1:2], 1.0 / n, stt[:, 3:4], ALU.mult, ALU.subtract)
    S.ts("dve", stt[:, 4:5], stt[:, 4:5], LN_EPS, None, ALU.add)
    S.act(stt[:, 5:6], stt[:, 4:5], AF.Sqrt)
    S.add("dve", lambda e: e.reciprocal(stt[:, 6:7], stt[:, 5:6]), reads=[stt[:, 5:6]], writes=[stt[:, 6:7]])
    S.stt("dve", stt[:, 7:8], stt[:, 2:3], -1.0, stt[:, 6:7], ALU.mult, ALU.mult)
    return stt[:, 6:7], stt[:, 7:8]


class Slots:
    def __init__(self, st, nc, name, dtype, nbanks, per_bank, psum=True):
        mk = nc.psum_tensor if psum else nc.sbuf_tensor
        self.s = []
        ts = [st.enter_context(mk("%s%d" % (name, b), [128, per_bank, 128], dtype)) for b in range(nbanks)]
        for k in range(per_bank):
            for t in ts:
                self.s.append(t[:, k, :])
        self.i = 0

    def get(self):
        s = self.s[self.i % len(self.s)]
        self.i += 1
        return s


def phase0(S, nc, A, P):
    from contextlib import ExitStack
    import os

    with ExitStack() as st:
        xt_pool = Pool(st, nc, "xt", [128, D], F32, 2)
        sq_pool = Pool(st, nc, "sq", [128, D], F32, 1)
        st_pool = Pool(st, nc, "lst", [128, 8], F32, 3)
        xn_pool = Pool(st, nc, "xn", [128, D], BF16, 2)
        hT_pool = Pool(st, nc, "hT", [128, KC, 128], BF16, 2)
        h0_pool = Pool(st, nc, "h0t", [128, D], F32, 2)
        ptr = Pool(st, nc, "ptr", [128, 8, 128], BF16, 2, psum=True)
        pba = Pool(st, nc, "pba", [128, 512], F32, 2, psum=True)
        wba = st.enter_context(nc.sbuf_tensor("wba", [128, KC, 64], BF16))
        if os.environ.get("KNOW", "0") != "1":
            S.dma("qpool", wba[:], A["w_in_v"][:, :, C_B : C_B + 64])
        gbi = st.enter_context(nc.sbuf_tensor("gbi", [128, 2, D], F32))
        S.dma("qsp", gbi[:], A["gb_in"])
        STOP = int(os.environ.get("KSTOP", "99"))
        for src, ntiles, dstT, ext in ((A["xb"], NCHK, A["h0T_d"], False), (A["xe"], EXT // 128, A["h0Te_d"], True)):
            if STOP < 2:
                break
            for t in range(ntiles if STOP > 10 else 1):
                xt = xt_pool.get()
                S.dma("qsp", xt[:], src[t * 128 : (t + 1) * 128, :])
                rstd, nmr = ln_stats(S, xt[:], D, st_pool, sq_pool)
                xn = xn_pool.get()
                S.act(xn[:], xt[:], AF.Identity, bias=nmr, scale=rstd)
                if STOP < 3:
                    continue
                hT = hT_pool.get()
                for half in range(2):
                    pt = ptr.get()
                    for c in range(8):
                        cc = half * 8 + c
                        S.tr(pt[:, c, :], xn[:, cc * 128 : (cc + 1) * 128], P["identb"][:])
                    for c in range(8):
                        cc = half * 8 + c
                        S.act(hT[:, cc, :], pt[:, c, :], AF.Identity, bias=P["lnin"][:, 1, cc : cc + 1], scale=P["lnin"][:, 0, cc : cc + 1])
                if STOP < 4:
                    continue
                S.dma("qsp", dstT.rearrange("c p t -> p c t")[:, :, t * 128 : (t + 1) * 128], hT[:])
                if STOP < 5:
                    continue
                if not ext:
                    pb = pba.get()
                    for c in range(KC):
                        S.mm(pb[:, 0:64], hT[:, c, :], wba[:, c, :], start=(c == 0), stop=(c == KC - 1))
                    S.copy("act", P["ba_tok"][:, t, :], pb[:, 0:64])
                else:
                    h0t = h0_pool.get()
                    S.act(h0t[:], xt[:], AF.Identity, bias=nmr, scale=rstd)
                    S.tt("dve", h0t[:], h0t[:], gbi[:, 0, :], ALU.mult)
                    S.tt("dve", h0t[:], h0t[:], gbi[:, 1, :], ALU.add)
                    S.dma("qsp", A["h0own_d"][t * 128 : (t + 1) * 128, :], h0t[:])


def phase1(S, nc, A, P):
    from contextlib import ExitStack
    import os
    KP1 = int(os.environ.get("KP1", "99"))
    NBLK = int(os.environ.get("KBLK", "8"))

    identb, identf, UIf, SLf, ILf, onesb, onesf = P["identb"], P["identf"], P["UIf"], P["SLf"], P["ILf"], P["onesb"], P["onesf"]
    with ExitStack() as st:
        sb = lambda n, s, d: st.enter_context(nc.sbuf_tensor(n, s, d))
        NC_ = NCHK * NH
        beta = sb("g_beta", [128, NC_], F32)
        nbeta = sb("g_nbeta", [128, NC_], F32)
        bg = sb("g_bg", [128, NC_], F32)
        eg = sb("g_eg", [128, NC_], F32)
        egl = sb("g_egl", [128, NC_], F32)
        ekd = sb("g_ekd", [128, NC_], F32)
        gstep = sb("g_step", [128, NC_], F32)
        gtmp = sb("g_tmp", [128, NC_], F32)
        ba = P["ba_tok"]
        v3 = lambda t: t[:].rearrange("p (n h) -> p n h", h=NH)
        S.act(v3(beta), ba[:, :, 0:32], AF.Sigmoid)
        S.ts("dve", nbeta[:], beta[:], -1.0, None, ALU.mult)
        S.tt("dve", v3(gtmp), ba[:, :, 32:64], P["gpar"][:, 1, :].rearrange("p (n h) -> p n h", h=NH), ALU.add)
        S.act(gtmp[:], gtmp[:], AF.Exp)
        S.act(gtmp[:], gtmp[:], AF.Ln, bias=1.0)
        S.act(gstep[:], P["gpar"][:, 0, :], AF.Exp)
        S.stt("dve", gstep[:], gtmp[:], -1.0, gstep[:], ALU.mult, ALU.mult)
        with ExitStack() as st2:
            pg = [st2.enter_context(nc.psum_tensor("pgs%d" % i, [128, 512], F32)) for i in range(4)]
            for hlf in range(2):
                cs = slice(hlf * 512, (hlf + 1) * 512)
                for q4 in range(4):
                    c4 = slice(hlf * 512 + q4 * 128, hlf * 512 + (q4 + 1) * 128)
                    S.mm(pg[hlf][:, q4 * 128 : (q4 + 1) * 128], UIf[:], gstep[:, c4])
                    S.mm(pg[2 + hlf][:, q4 * 128 : (q4 + 1) * 128], onesf[:], gstep[:, c4])
                S.copy("dve", gtmp[:, cs], pg[hlf][:])
                S.act(eg[:, cs], pg[hlf][:], AF.Exp)
                S.act(egl[:, cs], pg[2 + hlf][:], AF.Exp)
                S.tt("dve", ekd[:, cs], pg[2 + hlf][:], gtmp[:, cs], ALU.subtract)
                S.act(ekd[:, cs], ekd[:, cs], AF.Exp)
            S.tt("dve", bg[:], beta[:], eg[:], ALU.mult)
        S.barrier()

        w_pool = Pool(st, nc, "gw", [128, KC, 512], BF16, 1)
        hb_pool = Pool(st, nc, "ghb", [128, KC, 512], BF16, 2)
        xpre = [sb("xpre%d" % i, [128, 515], F32) for i in range(4)]
        cacc = Pool(st, nc, "cacc", [128, 512], F32, 3)
        xsf = Pool(st, nc, "xsf", [128, 512], F32, 3)
        vsb = Pool(st, nc, "vsb", [128, 512], BF16, 4)
        sqb = Pool(st, nc, "sqb", [128, 512], BF16, 2)
        rsp = Pool(st, nc, "rsp", [128, 512], F32, 2)
        qkn = Pool(st, nc, "qkn", [128, 512], BF16, 4)
        NCHAIN = 8
        clong, cshort, ckk = [], [], []
        for ch in range(NCHAIN):
            tb = sb("clb%d" % ch, [128, 6, 128], BF16)
            tf = sb("clf%d" % ch, [128, 3, 128], F32)
            clong.append({"attnT": tb[:, 0, :], "wT": tb[:, 1, :], "qd": tb[:, 2, :], "kbg": tb[:, 3, :], "kdec": tb[:, 4, :], "vb": tb[:, 5, :],
                          "Gm": tf[:, 0, :], "Dm": tf[:, 1, :], "u": tf[:, 2, :]})
            cshort.append(Slots(st, nc, "csh%d_" % ch, BF16, 1, 9, psum=False))
        for c in range(4):
            tk = sb("ckk%d" % c, [128, 2, 128], F32)
            ckk.append((tk[:, 0, :], tk[:, 1, :]))
        cvn = [Slots(st, nc, "cvn%d_" % hh, BF16, 1, 2, psum=False) for hh in range(2)]
        Sf = sb("Sf", [128, 2, 128], F32)
        Sb = sb("Sb", [128, 2, 128], BF16)
        oacc_pool = Pool(st, nc, "oacc", [128, 8, 2, 128], F32, 1)
        pproj = Pool(st, nc, "pproj", [128, 512], F32, 2, psum=True)
        ptb = Slots(st, nc, "ptb", BF16, 2, 8)
        psm = Slots(st, nc, "psm", F32, 4, 4)
        cw = P["cw"]
        h0T_v = A["h0T_d"].rearrange("c p t -> p c t")
        o_v = A["o_own_d"].rearrange("(l p) (h d) -> p l h d", p=128, d=128)

        for g in range(GDN_GROUPS if KP1 >= 2 else 0):
            wt = w_pool.get()
            S.dma("qpool", wt[:, :, 0:128], A["w_in_v"][:, :, C_Q + 128 * g : C_Q + 128 * (g + 1)])
            S.dma("qpool", wt[:, :, 128:256], A["w_in_v"][:, :, C_K + 128 * g : C_K + 128 * (g + 1)])
            S.dma("qpool", wt[:, :, 256:512], A["w_in_v"][:, :, C_V + 256 * g : C_V + 256 * (g + 1)])
            for ci in range(4):
                S.memset("dve", xpre[ci][:, 0:3], 0.0)
            S.memset("dve", Sf[:], 0.0)
            S.memset("dve", Sb[:], 0.0)
            oacc = oacc_pool.get()
            S.memset("dve", oacc[:], 0.0)
            cwi = [g, 16 + g, 32 + 2 * g, 33 + 2 * g]
            for blk in range(NBLK):
                hb = hb_pool.get()
                S.dma("qsp", hb[:], h0T_v[:, :, blk * 512 : (blk + 1) * 512])
                outs = []
                for ci in range(4):
                    pp = pproj.get()
                    for c in range(KC):
                        S.mm(pp[:], wt[:, c, ci * 128 : (ci + 1) * 128], hb[:, c, :], start=(c == 0), stop=(c == KC - 1))
                    xp = xpre[ci]
                    S.copy("act", xp[:, 3:515], pp[:])
                    acc = cacc.get()
                    S.ts("dve", acc[:], xp[:, 3:515], cw[:, cwi[ci], 3:4], None, ALU.mult)
                    for i in range(3):
                        S.stt("dve", acc[:], xp[:, i : i + 512], cw[:, cwi[ci], i : i + 1], acc[:], ALU.mult, ALU.add)
                    S.copy("dve", xp[:, 0:3], xp[:, 512:515])
                    if ci < 2:
                        xs = xsf.get()
                        S.act(xs[:], acc[:], AF.Silu)
                        sq = sqb.get()
                        S.act(sq[:], xs[:], AF.Square)
                        pss = pproj.get()
                        S.mm(pss[:], onesb[:], sq[:])
                        rs = rsp.get()
                        if ci == 0:
                            S.act(rs[:], pss[:], AF.Sqrt, bias=128.0 * RMS_EPS, scale=128.0)
                        else:
                            S.act(rs[:], pss[:], AF.Sqrt, bias=RMS_EPS, scale=1.0)
                        S.add("dve", lambda e, rs=rs: e.reciprocal(rs[:], rs[:]), reads=[rs[:]], writes=[rs[:]])
                        xn = qkn.get()
                        S.tt("dve", xn[:], xs[:], rs[:], ALU.mult)
                        outs.append(xn)
                    else:
                        vs = vsb.get()
                        S.act(vs[:], acc[:], AF.Silu)
                        outs.append(vs)
                qn, kn, vs0, vs1 = outs
                vss = (vs0, vs1)
                ctx = {}
                for c in range(4):
                    n = blk * 4 + c
                    csl = slice(c * 128, (c + 1) * 128)
                    kc_ = kn[:, csl]
                    qc_ = qn[:, csl]
                    pKK = psm.get()
                    S.mm(pKK, kc_, kc_)
                    KKm = ckk[c][0]
                    S.tt("dve", KKm, pKK, SLf[:], ALU.mult)
                    pQK = psm.get()
                    S.mm(pQK, qc_, kc_)
                    QKm = ckk[c][1]
                    S.tt("dve", QKm, pQK, ILf[:], ALU.mult)
                    pkt = ptb.get()
                    S.tr(pkt, kc_, identb[:])
                    for hh in range(2):
                        col = n * NH + 2 * g + hh
                        L = clong[c * 2 + hh]
                        S.act(L["kbg"], pkt, AF.Identity, scale=bg[:, col : col + 1])
                        S.act(L["kdec"], pkt, AF.Identity, scale=ekd[:, col : col + 1])
                    for hh in range(2):
                        col = n * NH + 2 * g + hh
                        L = clong[c * 2 + hh]
                        pvt = ptb.get()
                        S.tr(pvt, vss[hh][:, csl], identb[:])
                        S.act(L["vb"], pvt, AF.Identity, scale=beta[:, col : col + 1])

                def pre_chain(c, hh):
                    n = blk * 4 + c
                    csl = slice(c * 128, (c + 1) * 128)
                    qc_ = qn[:, csl]
                    col = n * NH + 2 * g + hh
                    cs1 = slice(col, col + 1)
                    ch = c * 2 + hh
                    L = clong[ch]
                    sh = cshort[ch]
                    KKm, QKm = ckk[c]
                    Gm, Dm = L["Gm"], L["Dm"]
                    S.ts("dve", Gm, UIf[:], gstep[:, cs1], None, ALU.mult)
                    yield
                    pD = psm.get()
                    S.mm(pD, Gm, SLf[:])
                    S.act(Dm, pD, AF.Exp)
                    yield
                    N = sh.get()
                    S.stt("dve", N, KKm, nbeta[:, cs1], Dm, ALU.mult, ALU.mult)
                    At = sh.get()
                    S.tt("dve", At, QKm, Dm, ALU.mult)
                    yield
                    pNT = ptb.get()
                    S.tr(pNT, N, identb[:])
                    NT = sh.get()
                    S.copy("act", NT, pNT)
                    PT = sh.get()
                    S.tt("dve", PT, pNT, identb[:], ALU.add)
                    pAT = ptb.get()
                    S.tr(pAT, At, identb[:])
                    S.copy("act", L["attnT"], pAT)
                    yield
                    Am, AT = N, NT
                    for lvl in range(6):
                        pA2 = psm.get()
                        S.mm(pA2, AT, Am)
                        A2 = sh.get()
                        S.copy("act", A2, pA2)
                        A2T = None
                        if lvl < 5:
                            pA2T = psm.get()
                            S.mm(pA2T, Am, AT)
                            A2T = sh.get()
                            S.copy("dve", A2T, pA2T)
                        yield
                        pP = psm.get()
                        S.mm(pP, A2, PT)
                        PTn = sh.get()
                        S.tt("dve", PTn, pP, PT, ALU.add)
                        PT = PTn
                        Am, AT = A2, A2T
                        yield
                    pu = psm.get()
                    S.mm(pu, PT, L["vb"])
                    S.copy("act", L["u"], pu)
                    pw = psm.get()
                    S.mm(pw, L["kbg"], PT)
                    S.copy("act", L["wT"], pw)
                    dg = sh.get()
                    S.ts("dve", dg, identb[:], eg[:, cs1], None, ALU.mult)
                    yield
                    pe_ = psm.get()
                    S.mm(pe_, onesb[:], dg)
                    S.tt("dve", L["qd"], pe_, qc_, ALU.mult)
                    yield

                def scan_chain(c, hh):
                    n = blk * 4 + c
                    col = n * NH + 2 * g + hh
                    cs1 = slice(col, col + 1)
                    L = clong[c * 2 + hh]
                    pws = psm.get()
                    S.mm(pws, L["wT"], Sb[:, hh, :])
                    vn = cvn[hh].get()
                    S.tt("dve", vn, L["u"], pws, ALU.subtract)
                    yield
                    po = psm.get()
                    S.mm(po, L["qd"], Sb[:, hh, :], start=True, stop=False)
                    S.mm(po, L["attnT"], vn, start=False, stop=True)
                    pdS = psm.get()
                    S.mm(pdS, L["kdec"], vn)
                    yield
                    oa = oacc[:, n % 8, hh, :]
                    S.stt("dve", oa, po, P["mseg"][:, n // 8 : n // 8 + 1], oa, ALU.mult, ALU.add)
                    S.stt("dve", Sf[:, hh, :], Sf[:, hh, :], egl[:, cs1], pdS, ALU.mult, ALU.add)
                    S.copy("act", Sb[:, hh, :], Sf[:, hh, :])
                    yield

                def run_interleaved(gens):
                    gens = list(gens)
                    while gens:
                        nxt = []
                        for gg in gens:
                            try:
                                next(gg)
                                nxt.append(gg)
                            except StopIteration:
                                pass
                        gens = nxt

                run_interleaved([pre_chain(c, hh) for c in range(4) for hh in range(2)])
                for c in range(4):
                    run_interleaved([scan_chain(c, hh) for hh in range(2)])
            S.dma("qsp", o_v[:, :, 2 * g : 2 * g + 2, :], oacc[:])


def phase2(S, nc, A, P):
    from contextlib import ExitStack

    identb, identf = P["identb"], P["identf"]
    wiv = A["w_in_v"]
    with ExitStack() as st:
        bank = Pool(st, nc, "pb2", [128, 512], F32, 6, psum=True)
        ptr = Pool(st, nc, "ptr2", [128, 8, 128], BF16, 1, psum=True)
        ptf = Slots(st, nc, "ptf2", F32, 1, 4)
        for hf in range(2):
            t0 = 128 + hf * 512
            with ExitStack() as sh:
                sbh = lambda n, s, d: sh.enter_context(nc.sbuf_tensor("%s_h%d" % (n, hf), s, d))
                h0w = sbh("h0w", [128, KC, 640], BF16)
                S.dma("qsp", h0w[:], A["h0Te_d"].rearrange("c p t -> p c t")[:, :, t0 - 128 : t0 + 512])
                onT = sbh("onT", [128, 32, 512], BF16)
                pT = sbh("pT", [128, KC, 512], BF16)
                mT = sbh("mT", [128, KC, 512], BF16)
                hw_own = lambda c: h0w[:, c, 128:640]
                with ExitStack() as sa:
                    nm = lambda n: "%s_a%d" % (n, hf)
                    on0 = [sa.enter_context(nc.sbuf_tensor(nm("on0_%d" % i), [128, 4096], BF16)) for i in range(4)]
                    big = Pool(sa, nc, nm("big"), [128, 4096], F32, 1)
                    ss_pool = Pool(sa, nc, nm("ss"), [128, 32], F32, 2)
                    wpool = Pool(sa, nc, nm("wz"), [128, KC, 512], BF16, 2)
                    f512 = Pool(sa, nc, nm("f5"), [128, 512], F32, 4)
                    for tt_ in range(4):
                        tile = hf * 4 + tt_
                        ot = big.get()
                        S.dma("qsp", ot[:], A["o_own_d"][tile * 128 : (tile + 1) * 128, :])
                        S.act(on0[tt_][:], ot[:], AF.Square)
                        ss = ss_pool.get()
                        S.add("dve", lambda e, ss=ss, sq=on0[tt_]: e.reduce_sum(ss[:], sq[:].rearrange("p (h d) -> p h d", d=128), AX.X), reads=[on0[tt_][:]], writes=[ss[:]])
                        S.act(ss[:], ss[:], AF.Sqrt, bias=RMS_EPS, scale=1.0 / 128.0)
                        S.add("dve", lambda e, ss=ss: e.reciprocal(ss[:], ss[:]), reads=[ss[:]], writes=[ss[:]])
                        for h in range(NH):
                            S.stt("dve", on0[tt_][:, h * 128 : (h + 1) * 128], ot[:, h * 128 : (h + 1) * 128], ss[:, h : h + 1], P["wn_bc"][:], ALU.mult, ALU.mult)
                    for zb in range(8):
                        wz = wpool.get()
                        S.dma("qpool", wz[:], wiv[:, :, C_Z + zb * 512 : C_Z + (zb + 1) * 512])
                        for tt_ in range(4):
                            pz = bank.get()
                            cb = 128 + tt_ * 128
                            for c in range(KC):
                                S.mm(pz[:], h0w[:, c, cb : cb + 128], wz[:, c, :], start=(c == 0), stop=(c == KC - 1))
                            sz = f512.get()
                            S.act(sz[:], pz[:], AF.Silu)
                            dst = on0[tt_][:, zb * 512 : (zb + 1) * 512]
                            S.tt("dve", dst, dst, sz[:], ALU.mult)
                    for tt_ in range(4):
                        for h8 in range(4):
                            pt = ptr.get()
                            for c in range(8):
                                hc = h8 * 8 + c
                                S.tr(pt[:, c, :], on0[tt_][:, hc * 128 : (hc + 1) * 128], identb[:])
                            S.copy("act", onT[:, h8 * 8 : (h8 + 1) * 8, tt_ * 128 : (tt_ + 1) * 128], pt[:])
                S.barrier()
                with ExitStack() as sa:
                    nm = lambda n: "%s_b%d" % (n, hf)
                    wpool = Pool(sa, nc, nm("wsc"), [128, KC, 384], BF16, 2)
                    f512 = Pool(sa, nc, nm("f5"), [128, 512], F32, 4)
                    u_pool = Pool(sa, nc, nm("u"), [128, 514], F32, 2)
                    hs_pool = Pool(sa, nc, nm("hs"), [128, 4], F32, 2)
                    for fc in range(KC):
                        wsc = wpool.get()
                        for k3, cbase in enumerate((C_SB, C_SC, C_SX)):
                            S.dma("qpool", wsc[:, :, k3 * 128 : (k3 + 1) * 128], wiv[:, :, cbase + fc * 128 : cbase + (fc + 1) * 128])
                        pc, px, pb_ = bank.get(), bank.get(), bank.get()
                        ph = ptf.get()
                        for c in range(KC):
                            S.mm(pc[:], wsc[:, c, 128:256], hw_own(c), start=(c == 0), stop=(c == KC - 1))
                        for c in range(KC):
                            S.mm(px[:], wsc[:, c, 256:384], hw_own(c), start=(c == 0), stop=(c == KC - 1))
                        for c in range(KC):
                            S.mm(pb_[:], wsc[:, c, 0:128], hw_own(c), start=(c == 0), stop=(c == KC - 1))
                        for c in range(KC):
                            S.mm(ph[:, 0:2], wsc[:, c, 128:256], h0w[:, c, 126:128], start=(c == 0), stop=(c == KC - 1))
                        for c in range(KC):
                            S.mm(ph[:, 2:4], wsc[:, c, 256:384], h0w[:, c, 126:128], start=(c == 0), stop=(c == KC - 1))
                        ccs = f512.get()
                        S.copy("act", ccs[:], pc[:])
                        u = u_pool.get()
                        S.tt("dve", u[:, 2:514], px[:], ccs[:], ALU.mult)
                        hs = hs_pool.get()
                        S.copy("act", hs[:], ph[:, 0:4])
                        S.tt("dve", u[:, 0:2], hs[:, 0:2], hs[:, 2:4], ALU.mult)
                        if hf == 0:
                            S.tt("dve", u[:, 0:2], u[:, 0:2], P["mh"][:], ALU.mult)
                        acc = f512.get()
                        scw = P["scw"]
                        S.ts("dve", acc[:], u[:, 2:514], scw[:, fc, 2:3], None, ALU.mult)
                        S.stt("dve", acc[:], u[:, 1:513], scw[:, fc, 1:2], acc[:], ALU.mult, ALU.add)
                        S.stt("dve", acc[:], u[:, 0:512], scw[:, fc, 0:1], acc[:], ALU.mult, ALU.add)
                        S.tt("dve", pT[:, fc, :], pb_[:], acc[:], ALU.mult)
                S.barrier()
                with ExitStack() as sa:
                    nm = lambda n: "%s_c%d" % (n, hf)
                    wm_pool = Pool(sa, nc, nm("wm"), [128, 80, 128], BF16, 2)
                    f512 = Pool(sa, nc, nm("f5"), [128, 512], F32, 4)
                    for fc in range(KC):
                        wm = wm_pool.get()
                        fs = slice(fc * 128, (fc + 1) * 128)
                        S.dma("qpool", wm[:, 0:32, :], A["w_out_gdn"].rearrange("(c p) n -> p c n", p=128)[:, :, fs])
                        S.dma("qpool", wm[:, 32:48, :], A["w_out_sc"].rearrange("(c p) n -> p c n", p=128)[:, :, fs])
                        S.dma("qpool", wm[:, 48:64, :], wiv[:, :, C_GA + fc * 128 : C_GA + (fc + 1) * 128])
                        S.dma("qpool", wm[:, 64:80, :], wiv[:, :, C_GB + fc * 128 : C_GB + (fc + 1) * 128])
                        pya, pga, pyb, pgb = bank.get(), bank.get(), bank.get(), bank.get()
                        for c in range(32):
                            S.mm(pya[:], wm[:, c, :], onT[:, c, :], start=(c == 0), stop=(c == 31))
                        for c in range(KC):
                            S.mm(pga[:], wm[:, 48 + c, :], hw_own(c), start=(c == 0), stop=(c == KC - 1))
                        for c in range(KC):
                            S.mm(pyb[:], wm[:, 32 + c, :], pT[:, c, :], start=(c == 0), stop=(c == KC - 1))
                        for c in range(KC):
                            S.mm(pgb[:], wm[:, 64 + c, :], hw_own(c), start=(c == 0), stop=(c == KC - 1))
                        sga, sgb = f512.get(), f512.get()
                        S.act(sga[:], pga[:], AF.Sigmoid)
                        S.tt("dve", sga[:], pya[:], sga[:], ALU.mult)
                        S.act(sgb[:], pgb[:], AF.Sigmoid)
                        S.tt("dve", sgb[:], pyb[:], sgb[:], ALU.mult)
                        S.tt("dve", mT[:, fc, :], sga[:], sgb[:], ALU.add)
                S.barrier()
                with ExitStack() as sa:
                    nm = lambda n: "%s_d%d" % (n, hf)
                    wpool = Pool(sa, nc, nm("wo"), [128, KC, 512], BF16, 2)
                    h1t = sa.enter_context(nc.sbuf_tensor(nm("h1t"), [128, 4, D], F32))
                    gbm = sa.enter_context(nc.sbuf_tensor(nm("gbm"), [128, 2, D], F32))
                    S.dma("qsp", gbm[:], A["gb_mix"])
                    lsq = Pool(sa, nc, nm("lsq"), [128, D], F32, 1)
                    st_pool = Pool(sa, nc, nm("lst"), [128, 8], F32, 3)
                    tmpT_pool = Pool(sa, nc, nm("tmpT"), [128, KC, 128], F32, 1)
                    hTs_pool = Pool(sa, nc, nm("hTs"), [128, KC, 128], BF16, 2)
                    lg_pool = Pool(sa, nc, nm("lg"), [128, NE], F32, 2)
                    for tt_ in range(4):
                        tile = hf * 4 + tt_
                        S.dma("qsp", h1t[:, tt_, :], A["h0own_d"][128 + tile * 128 : 128 + (tile + 1) * 128, :])
                    for cb in range(4):
                        wo = wpool.get()
                        S.dma("qpool", wo[:], A["w_out"].rearrange("(c p) n -> p c n", p=128)[:, :, cb * 512 : (cb + 1) * 512])
                        for tt_ in range(4):
                            pm = bank.get()
                            for c in range(KC):
                                S.mm(pm[:], mT[:, c, tt_ * 128 : (tt_ + 1) * 128], wo[:, c, :], start=(c == 0), stop=(c == KC - 1))
                            dst = h1t[:, tt_, cb * 512 : (cb + 1) * 512]
                            S.stt("dve", dst, dst, ALPHA, pm[:], ALU.mult, ALU.add)
                    for tt_ in range(4):
                        tile = hf * 4 + tt_
                        hv = h1t[:, tt_, :]
                        rstd, nmr = ln_stats(S, hv, D, st_pool, lsq)
                        S.act(hv, hv, AF.Identity, bias=nmr, scale=rstd)
                        S.tt("dve", hv, hv, gbm[:, 0, :], ALU.mult)
                        S.tt("dve", hv, hv, gbm[:, 1, :], ALU.add)
                        S.dma("qsp", A["h1_d"][tile * 128 : (tile + 1) * 128, :], hv)
                        tmpT = tmpT_pool.get()
                        hTs = hTs_pool.get()
                        for c in range(KC):
                            pt = ptf.get()
                            S.tr(pt, h1t[:, tt_, c * 128 : (c + 1) * 128], identf[:])
                            S.copy("act", hTs[:, c, :], pt)
                            S.copy("dve", tmpT[:, c, :], pt)
                        S.dma("qsp", A["h1T_d"].rearrange("c p t -> p c t")[:, :, tile * 128 : (tile + 1) * 128], hTs[:])
                        plg = ptf.get()
                        for c in range(KC):
                            S.mm(plg[:, 0:NE], tmpT[:, c, :], P["wr"][:, c, :], start=(c == 0), stop=(c == KC - 1))
                        lg = lg_pool.get()
                        S.tt("dve", lg[:], plg[:, 0:NE], P["br_bc"][:], ALU.add)
                        S.dma("qsp", A["lg_d"][tile * 128 : (tile + 1) * 128, :], lg[:])
                S.barrier()
        S.barrier()


def phase3(S, nc, A, P):
    from contextlib import ExitStack

    identf = P["identf"]
    with ExitStack() as st:
        sb = lambda n, s, d: st.enter_context(nc.sbuf_tensor(n, s, d))
        h1 = sb("h1", [128, 8, D], F32)
        h1T = sb("h1T", [128, KC, OWN], BF16)
        logits = sb("logits", [128, 8, NE], F32)
        S.dma("qsp", h1[:], A["h1_d"].rearrange("(t p) d -> p t d", p=128))
        S.dma("qsp", h1T[:], A["h1T_d"].rearrange("c p t -> p c t"))
        S.dma("qsp", logits[:], A["lg_d"].rearrange("(t p) e -> p t e", p=128))
        G = sb("G", [128, 8, NE], F32)
        st_outer = st
        st = st_outer.enter_context(ExitStack())
        sb = lambda n, s, d: st.enter_context(nc.sbuf_tensor(n, s, d))
        GT = sb("GT", [NE, 8, 128], F32)
        bd = sb("bd", [NE, D], F32)
        S.dma("qsp", bd[:], A["b_down"])
        bgu = sb("bgu_sb", [128, NE, KC, 2], F32)
        S.dma("qsp", bgu[:], A["bgu"])
        m8 = sb("m8", [128, 8, 8], F32)
        sm = sb("rt_sm", [128, 8, 4], F32)
        em = sb("rt_em", [128, 8, NE], F32)
        mk = sb("rt_mk", [128, 8, NE], F32)
        bank = Pool(st, nc, "pb3", [128, 512], F32, 7, psum=True)
        ptf = Slots(st, nc, "ptf3", F32, 1, 4)
        wpool = Pool(st, nc, "w3", [128, KC, 512], BF16, 2)
        actT = sb("actT", [128, KC, OWN], BF16)
        f512 = Pool(st, nc, "g512", [128, 512], F32, 4)
        for t in range(8):
            lg = logits[:, t, :]
            S.add("dve", lambda e, t=t, lg=lg: e.max(m8[:, t, :], lg), reads=[lg], writes=[m8[:, t, :]])
            S.ts("dve", mk[:, t, :], lg, m8[:, t, 3:4], None, ALU.is_ge)
            S.ts("dve", sm[:, t, 0:1], m8[:, t, 0:1], -1.0, None, ALU.mult)
            S.act(em[:, t, :], lg, AF.Exp, bias=sm[:, t, 0:1], scale=1.0)
            S.tt("dve", em[:, t, :], em[:, t, :], mk[:, t, :], ALU.mult)
            S.add("dve", lambda e, t=t: e.reduce_sum(sm[:, t, 1:2], em[:, t, :], AX.X), reads=[em[:, t, :]], writes=[sm[:, t, 1:2]])
            S.add("dve", lambda e, t=t: e.reciprocal(sm[:, t, 2:3], sm[:, t, 1:2]), reads=[sm[:, t, 1:2]], writes=[sm[:, t, 2:3]])
            S.ts("dve", G[:, t, :], em[:, t, :], sm[:, t, 2:3], None, ALU.mult)
            pt = ptf.get()
            S.tr(pt[0:NE, :], G[:, t, :], identf[:])
            S.copy("act", GT[:, t, :], pt[0:NE, :])
            for cb in range(4):
                pb = bank.get()
                for q4 in range(4):
                    S.mm(pb[:, q4 * 128 : (q4 + 1) * 128], GT[:, t, :], bd[:, cb * 512 + q4 * 128 : cb * 512 + (q4 + 1) * 128])
                dst = h1[:, t, cb * 512 : (cb + 1) * 512]
                S.stt("dve", dst, dst, ALPHA, pb[:], ALU.mult, ALU.add)
        for e in range(NE):
            wgu_v = A["w_gate_up"][e].rearrange("(c p) n -> p c n", p=128)
            wd_v = A["w_down"][e].rearrange("(c p) n -> p c n", p=128)
            for gb in range(8):
                wg = wpool.get()
                S.dma("qpool", wg[:], wgu_v[:, :, gb * 512 : (gb + 1) * 512])
                for sub in range(2):
                    fc = gb * 2 + sub
                    for th in range(2):
                        ts_ = slice(th * 512, (th + 1) * 512)
                        pg, pu = bank.get(), bank.get()
                        for c in range(KC):
                            S.mm(pg[:], wg[:, c, sub * 256 : sub * 256 + 256 : 2], h1T[:, c, ts_], start=(c == 0), stop=(c == KC - 1))
                        for c in range(KC):
                            S.mm(pu[:], wg[:, c, sub * 256 + 1 : sub * 256 + 256 : 2], h1T[:, c, ts_], start=(c == 0), stop=(c == KC - 1))
                        gt = f512.get()
                        S.ts("dve", gt[:], pg[:], bgu[:, e, fc, 0:1], 7.0, ALU.add, ALU.min)
                        sg = f512.get()
                        S.act(sg[:], gt[:], AF.Sigmoid, scale=1.702)
                        up = f512.get()
                        S.ts("dve", up[:], pu[:], bgu[:, e, fc, 1:2], 7.0, ALU.add, ALU.min)
                        S.ts("dve", up[:], up[:], -7.0, 1.0, ALU.max, ALU.add)
                        S.tt("dve", gt[:], gt[:], sg[:], ALU.mult)
                        S.tt("dve", actT[:, fc, ts_], gt[:], up[:], ALU.mult)
            for cb in range(4):
                wd = wpool.get()
                S.dma("qpool", wd[:], wd_v[:, :, cb * 512 : (cb + 1) * 512])
                for t in range(8):
                    pd = bank.get()
                    for c in range(KC):
                        S.mm(pd[:], actT[:, c, t * 128 : (t + 1) * 128], wd[:, c, :], start=(c == 0), stop=(c == KC - 1))
                    dst = h1[:, t, cb * 512 : (cb + 1) * 512]
                    S.stt("dve", dst, pd[:], G[:, t, e : e + 1], dst, ALU.mult, ALU.add)
        S.barrier()
        st.close()
        st = st_outer
        sb = lambda n, s, d: st.enter_context(nc.sbuf_tensor(n, s, d))
        gbf = sb("gbf", [128, 2, D], F32)
        S.dma("qsp", gbf[:], A["gb_ffn"])
        st_pool = Pool(st, nc, "lst3", [128, 8], F32, 3)
        sq_pool = Pool(st, nc, "sq3", [128, D], F32, 1)
        o_pool = Pool(st, nc, "o3", [128, D], F32, 2)
        for t in range(8):
            hv = h1[:, t, :]
            rstd, nmr = ln_stats(S, hv, D, st_pool, sq_pool)
            ot = o_pool.get()
            S.act(ot[:], hv, AF.Identity, bias=nmr, scale=rstd)
            S.tt("dve", ot[:], ot[:], gbf[:, 0, :], ALU.mult)
            S.tt("dve", ot[:], ot[:], gbf[:, 1, :], ALU.add)
            S.dma("qsp", A["out"][t * 128 : (t + 1) * 128, :], ot[:])


def build_program():
    from contextlib import ExitStack

    nc = bass.Bass("TRN2", target_bir_lowering=False)
    A = {}

    def din(name, shape):
        A[name] = nc.dram_tensor(name, shape, F32, kind="ExternalInput").ap()
        return A[name]

    din("xb", [SEQ, D])
    din("xe", [EXT, D])
    din("consts", [128, 5, 128])
    din("lnin", [128, 2, KC])
    din("gb_in", [128, 2, D])
    din("gb_mix", [128, 2, D])
    din("gb_ffn", [128, 2, D])
    din("gpar", [128, 2, NCHK * NH])
    din("cw", [128, 64, 4])
    din("scw", [128, KC, 3])
    din("wn_bc", [128, 128])
    din("mseg", [128, 4])
    din("mh", [128, 2])
    din("wr", [128, KC, NE])
    din("br_bc", [128, NE])
    import os
    NOW = os.environ.get("KNOW", "0") == "1"
    if not NOW:
        din("w_in", [D, 22592])
        din("w_out_gdn", [4096, D])
        din("w_out_sc", [D, D])
        din("w_out", [D, D])
    else:
        din("w_in", [128, 22592])
        A["w_in_v"] = bass.AP(A["w_in"].tensor, 0, [[22592, 128], [0, KC], [1, 22592]])
    if 3 in PHASES:
        din("w_gate_up", [NE, D, 4096])
        din("w_down", [NE, D, D])
    din("bgu", [128, NE, KC, 2])
    din("b_down", [NE, D])
    if not NOW:
        A["w_in_v"] = A["w_in"].rearrange("(c p) n -> p c n", p=128)
    A["out"] = nc.dram_tensor("out", [OWN, D], F32, kind="ExternalOutput").ap()
    kd = "ExternalOutput" if DEBUG else "Internal"
    A["h0T_d"] = nc.dram_tensor("h0T_d", [KC, 128, SEQ], BF16, kind=kd).ap()
    A["h0Te_d"] = nc.dram_tensor("h0Te_d", [KC, 128, EXT], BF16, kind=kd).ap()
    A["h0own_d"] = nc.dram_tensor("h0own_d", [EXT, D], F32, kind=kd).ap()
    A["o_own_d"] = nc.dram_tensor("o_own_d", [OWN, 4096], F32, kind=kd).ap()
    A["h1_d"] = nc.dram_tensor("h1_d", [OWN, D], F32, kind=kd).ap()
    A["h1T_d"] = nc.dram_tensor("h1T_d", [KC, 128, OWN], BF16, kind=kd).ap()
    A["lg_d"] = nc.dram_tensor("lg_d", [OWN, NE], F32, kind=kd).ap()

    S = Sched(nc)
    P = {}
    with ExitStack() as st:
        sb = lambda n, s, d: st.enter_context(nc.sbuf_tensor(n, s, d))
        cf = sb("cf", [128, 5, 128], F32)
        cb = sb("cb", [128, 5, 128], BF16)
        S.dma("qsp", cf[:], A["consts"])
        S.dma("qpool", cb[:], A["consts"])
        P["identf"], P["UIf"], P["SLf"], P["ILf"], P["onesf"] = (cf[:, i, :] for i in range(5))
        P["identb"], P["onesb"] = cb[:, 0, :], cb[:, 4, :]
        for name, shape in (("lnin", [128, 2, KC]), ("cw", [128, 64, 4]), ("scw", [128, KC, 3]), ("wn_bc", [128, 128]),
                            ("mseg", [128, 4]), ("mh", [128, 2]), ("wr", [128, KC, NE]), ("br_bc", [128, NE])):
            t = sb("c_" + name, shape, F32)
            S.dma("qsp", t[:], A[name])
            P[name] = t
        with ExitStack() as st01:
            P["ba_tok"] = st01.enter_context(nc.sbuf_tensor("ba_tok", [128, NCHK, 64], F32))
            P["gpar"] = st01.enter_context(nc.sbuf_tensor("gpar_sb", [128, 2, NCHK * NH], F32))
            S.dma("qsp", P["gpar"][:], A["gpar"])
            if 0 in PHASES:
                phase0(S, nc, A, P)
            S.barrier()
            if 1 in PHASES:
                phase1(S, nc, A, P)
            S.barrier()
        if 2 in PHASES:
            phase2(S, nc, A, P)
        S.barrier()
        if 3 in PHASES:
            phase3(S, nc, A, P)
        S.emit()
    return nc


def _prep_inputs(inp):
    f = lambda a: np.ascontiguousarray(a, dtype=np.float32)
    x = inp["x"]
    pc = lambda v: f(np.asarray(v).reshape(-1, 128).T)
    bc = lambda v: f(np.broadcast_to(np.asarray(v).reshape(1, -1), (128, np.asarray(v).size)))
    pidx = np.arange(128)[:, None]
    fidx = np.arange(128)[None, :]
    consts = np.stack([(pidx == fidx), (pidx <= fidx), (pidx > fidx), (pidx >= fidx), np.ones((128, 128), bool)], axis=1)
    common = {
        "consts": f(consts),
        "lnin": f(np.stack([pc(inp["ln_in_g"]), pc(inp["ln_in_b"])], axis=1)),
        "gb_in": f(np.stack([bc(inp["ln_in_g"]), bc(inp["ln_in_b"])], axis=1)),
        "gb_mix": f(np.stack([bc(inp["ln_mix_g"][0]), bc(inp["ln_mix_b"][0])], axis=1)),
        "gb_ffn": f(np.stack([bc(inp["ln_ffn_g"][0]), bc(inp["ln_ffn_b"][0])], axis=1)),
        "gpar": f(np.stack([bc(np.tile(inp["gdn_a_log"][0], NCHK)), bc(np.tile(inp["gdn_dt_bias"][0], NCHK))], axis=1)),
        "cw": f(np.asarray(inp["gdn_conv_w"][0]).T.reshape(64, 128, 4).transpose(1, 0, 2)),
        "scw": f(np.asarray(inp["sc_conv_w"][0]).T.reshape(KC, 128, 3).transpose(1, 0, 2)),
        "wn_bc": bc(inp["gdn_norm_w"][0]),
        "wr": f(np.asarray(inp["w_router"][0]).reshape(KC, 128, NE).transpose(1, 0, 2)),
        "br_bc": bc(inp["b_router"][0]),
        "w_in": f(inp["w_in"][0]),
        "w_out_gdn": f(inp["w_out_gdn"][0]),
        "w_out_sc": f(inp["w_out_sc"][0]),
        "w_out": f(inp["w_out"][0]),
        "w_gate_up": f(inp["w_gate_up"][0]),
        "w_down": f(inp["w_down"][0]),
        "bgu": f(np.asarray(inp["b_gate_up"][0]).reshape(NE, KC, 128, 2).transpose(2, 0, 1, 3)),
        "b_down": f(inp["b_down"][0]),
    }
    maps = []
    for c in range(8):
        b, j = c // 4, c % 4
        xe = np.zeros((EXT, D), np.float32)
        lo = OWN * j - 128
        if lo < 0:
            xe[128:] = x[b, 0:OWN]
        else:
            xe[:] = x[b, lo : lo + EXT]
        mseg = np.zeros((128, 4), np.float32)
        mseg[:, j] = 1.0
        mh = np.full((128, 2), 0.0 if j == 0 else 1.0, np.float32)
        m = dict(common)
        m.update({"xb": f(x[b]), "xe": xe, "mseg": mseg, "mh": mh})
        maps.append(m)
    return maps


_NC_CACHE = {}


def kernel(**inputs):
    inp = {k: np.asarray(v) for k, v in inputs.items()}
    if "nc" not in _NC_CACHE:
        _NC_CACHE["nc"] = build_program()
    nc = _NC_CACHE["nc"]
    maps = _prep_inputs(inp)
    res = run_bass_kernel_spmd(nc, maps, core_ids=list(range(8)))
    out = np.zeros((2, SEQ, D), np.float32)
    for c in range(8):
        b, j = c // 4, c % 4
        out[b, OWN * j : OWN * (j + 1)] = np.asarray(res.results[c]["out"], dtype=np.float32)
    if DEBUG:
        kernel.last = res
    return out
```
